# Optimizing a Trainium2 kernel written in Bass

```python
import jax, jax.numpy as jnp
from jax import lax
import numpy as np

D_MODEL = 1024
BATCH = 16
SEQ = 4096
DEPTH = 1

CHUNK = 64
N_META = 16
Q_BLOCK = 128
EPS = 1e-6
GLA_HEADS = 4
GLA_DK = 64
GLA_DV = 128
GLA_GATE_RANK = 16
GLA_TAU = 16.0
GLA_QK = GLA_HEADS * GLA_DK
GLA_VW = GLA_HEADS * GLA_DV
MLA_HEADS = 4
MLA_Q_RANK = 256
MLA_KV_RANK = 128
MLA_NOPE = 128
MLA_ROPE = 64
MLA_V = 128
MLA_OUT = MLA_HEADS * MLA_V
ROPE_BASE = 10000.0
D_MIX = GLA_VW + MLA_OUT
IN_WIDTHS = (GLA_QK, GLA_QK, GLA_VW, GLA_VW, GLA_GATE_RANK, MLA_Q_RANK, MLA_KV_RANK, MLA_ROPE)
D_IN = sum(IN_WIDTHS)
N_GROUPS = 8
EXPERTS_PER_GROUP = 8
N_EXPERTS = N_GROUPS * EXPERTS_PER_GROUP
TOP_K = 2
D_EXPERT = 512
MOE_BLOCK = 128

kernel_name = 'hymba_gla_mla_hier_moe'


def rmsnorm(x, gain):
    x32 = x.astype(jnp.float32)
    y = x32 * lax.rsqrt(jnp.mean(x32 * x32, axis=-1, keepdims=True) + EPS)
    return (y * gain.astype(jnp.float32)).astype(x.dtype)


def split_points():
    pts, acc = [], 0
    for w in IN_WIDTHS[:-1]:
        acc += w
        pts.append(acc)
    return pts


def chunk_ids(length):
    p = jnp.arange(length)
    return jnp.where(p < N_META, 0, 1 + (p - N_META) // CHUNK)


def rope_tables(length):
    pos = jnp.arange(length, dtype=jnp.float32)
    inv = ROPE_BASE ** (-jnp.arange(0, MLA_ROPE, 2, dtype=jnp.float32) / MLA_ROPE)
    ang = pos[:, None] * inv[None, :]
    return jnp.cos(ang), jnp.sin(ang)


def apply_rope(x, cos, sin):
    half = x.shape[-1] // 2
    x1, x2 = x[..., :half].astype(jnp.float32), x[..., half:].astype(jnp.float32)
    return jnp.concatenate([x1 * cos - x2 * sin, x2 * cos + x1 * sin], axis=-1).astype(x.dtype)


def gla_group(q, k, v, r, a_low, w_a2, b_a, out_gain):
    f32 = jnp.float32
    B, L, _ = q.shape
    pad = (-L) % CHUNK
    log_a = jax.nn.log_sigmoid((a_low @ w_a2 + b_a).astype(f32)) / GLA_TAU

    def prep(t, d):
        t = jnp.pad(t.astype(f32), ((0, 0), (pad, 0), (0, 0)))
        n = t.shape[1] // CHUNK
        return t.reshape(B, n, CHUNK, GLA_HEADS, d).transpose(0, 3, 1, 2, 4)

    qc = prep(q, GLA_DK) * (GLA_DK ** -0.5)
    kc = prep(k, GLA_DK)
    vc = prep(v, GLA_DV)
    b = jnp.cumsum(prep(log_a, GLA_DK), axis=3)
    b_last = b[:, :, :, -1:, :]
    q_e = qc * jnp.exp(b)
    causal = jnp.tril(jnp.ones((CHUNK, CHUNK), dtype=bool))
    att = jnp.where(causal, jnp.einsum('bhncd,bhnsd->bhncs', q_e, kc * jnp.exp(-b)), 0.0)
    o = jnp.einsum('bhncs,bhnse->bhnce', att, vc)
    upd = jnp.einsum('bhncd,bhnce->nbhde', kc * jnp.exp(b_last - b), vc)
    decay = jnp.exp(b_last[:, :, :, 0, :]).transpose(2, 0, 1, 3)

    def step(s, inp):
        dec, u = inp
        return dec[..., None] * s + u, s

    s0 = jnp.zeros((B, GLA_HEADS, GLA_DK, GLA_DV), f32)
    _, s_prev = lax.scan(step, s0, (decay, upd))
    o = o + jnp.einsum('bhncd,nbhde->bhnce', q_e, s_prev)
    o = o.transpose(0, 2, 3, 1, 4).reshape(B, -1, GLA_HEADS, GLA_DV)[:, pad:]
    o = o * lax.rsqrt(jnp.mean(o * o, axis=-1, keepdims=True) + EPS)
    o = o.reshape(B, L, GLA_VW) * out_gain.astype(f32)
    return (o * jax.nn.silu(r.astype(f32))).astype(q.dtype)


def mla_group(q_lat, kv_lat, k_rope, q_norm, w_qb, kv_norm, w_kvb):
    B, L, _ = q_lat.shape
    q = (rmsnorm(q_lat, q_norm) @ w_qb).reshape(B, L, MLA_HEADS, MLA_NOPE + MLA_ROPE)
    q_nope, q_rope = q[..., :MLA_NOPE], q[..., MLA_NOPE:]
    kv = (rmsnorm(kv_lat, kv_norm) @ w_kvb).reshape(B, L, MLA_HEADS, MLA_NOPE + MLA_V)
    k_nope, v = kv[..., :MLA_NOPE], kv[..., MLA_NOPE:]
    cos, sin = rope_tables(L)
    q_rope = apply_rope(q_rope, cos[:, None, :], sin[:, None, :])
    k_rope = apply_rope(k_rope, cos, sin)
    key_chunk = chunk_ids(L)
    scale = (MLA_NOPE + MLA_ROPE) ** -0.5

    def attend(args):
        qn, qr, qcid = args
        s = (jnp.einsum('bqhd,bkhd->bhqk', qn, k_nope)
             + jnp.einsum('bqhd,bkd->bhqk', qr, k_rope)).astype(jnp.float32) * scale
        s = jnp.where(key_chunk[None, None, None, :] <= qcid[None, None, :, None], s, -jnp.inf)
        p = jax.nn.softmax(s, axis=-1).astype(v.dtype)
        return jnp.einsum('bhqk,bkhd->bqhd', p, v)

    meta_out = attend((q_nope[:, :N_META], q_rope[:, :N_META], key_chunk[:N_META]))
    nb = (L - N_META) // Q_BLOCK

    def blocks(t):
        return t[:, N_META:].reshape(B, nb, Q_BLOCK, *t.shape[2:]).swapaxes(0, 1)

    blk = lax.map(attend, (blocks(q_nope), blocks(q_rope), key_chunk[N_META:].reshape(nb, Q_BLOCK)))
    blk = blk.swapaxes(0, 1).reshape(B, L - N_META, MLA_HEADS, MLA_V)
    return jnp.concatenate([meta_out, blk], axis=1).reshape(B, L, MLA_OUT)


def hier_moe(u, wg, bg, we, be, w_gate, w_up, w_down):
    f32 = jnp.float32
    B, L, D = u.shape
    t = u.reshape(-1, D)
    n_tok = t.shape[0]
    g_prob = jax.nn.softmax((t @ wg + bg).astype(f32), axis=-1)
    g_p, g_idx = lax.top_k(g_prob, 1)
    e_logits = (t @ we + be).astype(f32).reshape(n_tok, N_GROUPS, EXPERTS_PER_GROUP)
    e_logits = jnp.take_along_axis(e_logits, g_idx[:, :, None], axis=1)[:, 0]
    e_p, e_idx = lax.top_k(jax.nn.softmax(e_logits, axis=-1), TOP_K)
    gates = g_p * e_p / jnp.sum(e_p, axis=-1, keepdims=True)
    expert = g_idx * EXPERTS_PER_GROUP + e_idx
    e_flat = expert.reshape(-1)
    tok_flat = jnp.repeat(jnp.arange(n_tok, dtype=jnp.int32), TOP_K)
    gate_flat = gates.reshape(-1)
    order = jnp.argsort(e_flat)
    e_sorted, tok_sorted, gate_sorted = e_flat[order], tok_flat[order], gate_flat[order]
    counts = jnp.bincount(e_flat, length=N_EXPERTS)
    padded = (counts + MOE_BLOCK - 1) // MOE_BLOCK * MOE_BLOCK
    pad_end = jnp.cumsum(padded)
    pad_start = pad_end - padded
    seg_start = jnp.cumsum(counts) - counts
    n_assign = n_tok * TOP_K
    dest = pad_start[e_sorted] + jnp.arange(n_assign) - seg_start[e_sorted]
    n_blocks = -(-(n_assign + N_EXPERTS * (MOE_BLOCK - 1)) // MOE_BLOCK)
    n_slots = n_blocks * MOE_BLOCK
    slot_tok = jnp.full((n_slots,), n_tok, jnp.int32).at[dest].set(tok_sorted)
    slot_gate = jnp.zeros((n_slots,), f32).at[dest].set(gate_sorted)
    block_expert = jnp.minimum(
        jnp.searchsorted(pad_end, jnp.arange(n_blocks) * MOE_BLOCK, side='right'), N_EXPERTS - 1)
    t_pad = jnp.concatenate([t, jnp.zeros((1, D), t.dtype)], axis=0)
    xs = t_pad[slot_tok].reshape(n_blocks, MOE_BLOCK, D)

    def expert_block(args):
        xb, e = args
        hdn = jax.nn.silu(xb @ w_gate[e]) * (xb @ w_up[e])
        return hdn @ w_down[e]

    ys = lax.map(expert_block, (xs, block_expert)).reshape(n_slots, D)
    out = jnp.zeros((n_tok + 1, D), f32).at[slot_tok].add(ys.astype(f32) * slot_gate[:, None])[:n_tok]
    return out.reshape(B, L, D).astype(u.dtype)


def setup_inputs(seed: int = 0) -> dict:
    key = jax.random.key(seed)
    ks = jax.random.split(key, 24)
    f32 = jnp.float32

    def nrm(k, shape, scale):
        return jax.random.normal(k, shape, f32) * scale

    def gain(k, shape):
        return 1.0 + 0.02 * jax.random.normal(k, shape, f32)

    return {
        'x': nrm(ks[0], (BATCH, SEQ, D_MODEL), 1.0),
        'meta_tokens': nrm(ks[1], (N_META, D_MODEL), 1.0),
        'mix_norm': gain(ks[2], (DEPTH, D_MODEL)),
        'w_in': nrm(ks[3], (DEPTH, D_MODEL, D_IN), D_MODEL ** -0.5),
        'gla_w_a2': nrm(ks[4], (DEPTH, GLA_GATE_RANK, GLA_QK), GLA_GATE_RANK ** -0.5),
        'gla_b_a': nrm(ks[5], (DEPTH, GLA_QK), 0.1),
        'gla_out_norm': gain(ks[6], (DEPTH, GLA_VW)),
        'mla_q_norm': gain(ks[7], (DEPTH, MLA_Q_RANK)),
        'mla_w_qb': nrm(ks[8], (DEPTH, MLA_Q_RANK, MLA_HEADS * (MLA_NOPE + MLA_ROPE)), MLA_Q_RANK ** -0.5),
        'mla_kv_norm': gain(ks[9], (DEPTH, MLA_KV_RANK)),
        'mla_w_kvb': nrm(ks[10], (DEPTH, MLA_KV_RANK, MLA_HEADS * (MLA_NOPE + MLA_V)), MLA_KV_RANK ** -0.5),
        'w_out': nrm(ks[11], (DEPTH, D_MIX, D_MODEL), D_MIX ** -0.5),
        'ffn_norm': gain(ks[12], (DEPTH, D_MODEL)),
        'router_group_w': nrm(ks[13], (DEPTH, D_MODEL, N_GROUPS), D_MODEL ** -0.5),
        'router_group_b': nrm(ks[14], (DEPTH, N_GROUPS), 0.01),
        'router_expert_w': nrm(ks[15], (DEPTH, D_MODEL, N_EXPERTS), D_MODEL ** -0.5),
        'router_expert_b': nrm(ks[16], (DEPTH, N_EXPERTS), 0.01),
        'expert_w_gate': nrm(ks[17], (DEPTH, N_EXPERTS, D_MODEL, D_EXPERT), D_MODEL ** -0.5),
        'expert_w_up': nrm(ks[18], (DEPTH, N_EXPERTS, D_MODEL, D_EXPERT), D_MODEL ** -0.5),
        'expert_w_down': nrm(ks[19], (DEPTH, N_EXPERTS, D_EXPERT, D_MODEL), D_EXPERT ** -0.5),
        'final_norm': gain(ks[20], (D_MODEL,)),
    }


def reference(x, meta_tokens, mix_norm, w_in, gla_w_a2, gla_b_a, gla_out_norm, mla_q_norm, mla_w_qb,
              mla_kv_norm, mla_w_kvb, w_out, ffn_norm, router_group_w, router_group_b, router_expert_w,
              router_expert_b, expert_w_gate, expert_w_up, expert_w_down, final_norm):
    B = x.shape[0]
    meta = jnp.broadcast_to(meta_tokens[None].astype(x.dtype), (B, N_META, x.shape[-1]))
    h = jnp.concatenate([meta, x], axis=1)
    pts = split_points()
    for l in range(DEPTH):
        u = rmsnorm(h, mix_norm[l])
        z = u @ w_in[l]
        q, k, v, r, a_low, q_lat, kv_lat, k_rope = jnp.split(z, pts, axis=-1)
        y_gla = gla_group(q, k, v, r, a_low, gla_w_a2[l], gla_b_a[l], gla_out_norm[l])
        y_mla = mla_group(q_lat, kv_lat, k_rope, mla_q_norm[l], mla_w_qb[l], mla_kv_norm[l], mla_w_kvb[l])
        h = h + jnp.concatenate([y_gla, y_mla], axis=-1) @ w_out[l]
        h = h + hier_moe(rmsnorm(h, ffn_norm[l]), router_group_w[l], router_group_b[l], router_expert_w[l],
                         router_expert_b[l], expert_w_gate[l], expert_w_up[l], expert_w_down[l])
    return rmsnorm(h, final_norm)[:, N_META:]
```

```python
import numpy as np
from contextlib import ExitStack
import concourse.bass as bass
import concourse.mybir as mybir
from concourse.bass_utils import run_bass_kernel_spmd

F32 = mybir.dt.float32
BF16 = mybir.dt.bfloat16
I32 = mybir.dt.int32
AF = mybir.ActivationFunctionType
ALU = mybir.AluOpType
AX = mybir.AxisListType
ENGS = ("pe", "act", "dve", "pool", "sp")
EPS = 1e-6


class Buf:
    __slots__ = ("name", "lw", "rd")

    def __init__(self, name):
        self.name = name
        self.lw = None
        self.rd = {}


class Call:
    __slots__ = ("name", "args", "kwargs")

    def __init__(self, name, args, kwargs):
        self.name, self.args, self.kwargs = name, args, kwargs


class _Rec:
    def __getattr__(self, name):
        def f(*args, **kwargs):
            return Call(name, args, kwargs)
        return f


E = _Rec()


class Prog:
    def __init__(self, nc):
        self.nc = nc
        self.ops = {e: [] for e in ENGS}
        self.cnt = {e: 0 for e in ENGS}
        self.seen = {e: {} for e in ENGS}
        self.sems = {}
        self.dcnt = {}
        import os
        self.limit = int(os.environ.get("KLIMIT", "100000000"))

    def _deps(self, eng, reads, writes):
        ev = {}

        def need(e):
            if e is None:
                return
            k, v = e
            if ev.get(k, 0) < v:
                ev[k] = v
        for b in reads:
            need(b.lw)
        for b in writes:
            need(b.lw)
            for k, v in b.rd.items():
                need((k, v))
        waits = []
        for k, v in ev.items():
            if k == eng and eng in ("pe", "sp"):
                continue
            if self.seen[eng].get(k, 0) >= v:
                continue
            self.seen[eng][k] = v
            waits.append((k, v))
        return waits

    def _commit(self, event, reads, writes):
        k, v = event
        for b in reads:
            if b.rd.get(k, 0) < v:
                b.rd[k] = v
        for b in writes:
            b.lw = event
            b.rd = {}

    def op(self, eng, fn, reads=(), writes=()):
        self.nrec = getattr(self, "nrec", 0) + 1
        if self.nrec > self.limit:
            return
        waits = self._deps(eng, reads, writes)
        self.cnt[eng] += 1
        self.ops[eng].append((waits, fn, (eng, 1)))
        self._commit((eng, self.cnt[eng]), reads, writes)

    def dma(self, eng, dsem, fn, reads=(), writes=()):
        self.nrec = getattr(self, "nrec", 0) + 1
        if self.nrec > self.limit:
            return
        waits = self._deps(eng, reads, writes)
        self.dcnt[dsem] = self.dcnt.get(dsem, 0) + 16
        self.ops[eng].append((waits, fn, (dsem, 16)))
        self._commit((dsem, self.dcnt[dsem]), reads, writes)

    def barrier(self):
        for e in ENGS:
            waits = []
            for k in ENGS:
                if k != e and self.cnt[k] > self.seen[e].get(k, 0):
                    self.seen[e][k] = self.cnt[k]
                    waits.append((k, self.cnt[k]))
            for k, v in self.dcnt.items():
                if v > self.seen[e].get(k, 0):
                    self.seen[e][k] = v
                    waits.append((k, v))
            self.ops[e].append((waits, None, None))

    def emit(self, stack):
        nc = self.nc
        keys = set(ENGS) | set(self.dcnt.keys())
        for k in sorted(keys):
            self.sems[k] = stack.enter_context(nc.semaphore("s_" + k))
        stack.enter_context(nc.allow_non_contiguous_dma(reason="tiny gain vectors / router weights / broadcasts"))
        block = stack.enter_context(nc.Block())
        sems = self.sems

        def runner(e):
            lst = self.ops[e]

            def body(engobj):
                for waits, fn, inc in lst:
                    for k, v in waits:
                        engobj.wait_ge(sems[k], v)
                    if fn is not None:
                        getattr(engobj, fn.name)(*fn.args, **fn.kwargs).then_inc(sems[inc[0]], inc[1])
            return body
        block.tensor(runner("pe"))
        block.scalar(runner("act"))
        block.vector(runner("dve"))
        block.gpsimd(runner("pool"))
        block.sync(runner("sp"))


class Arena:
    def __init__(self, nc, stack, nbytes):
        self.t = stack.enter_context(nc.sbuf_tensor("arena", [128, nbytes // 4], F32))
        self.nbytes = nbytes
        self.off = 0
        self.marks = []
        self.peak = 0

    def alloc(self, shape, dtype, parts=128):
        esz = 2 if dtype == BF16 else 4
        n = int(np.prod(shape))
        nb = (n * esz + 63) // 64 * 64
        assert self.off + nb <= self.nbytes, f"arena overflow {self.off}+{nb}>{self.nbytes}"
        a, b = self.off // 4, (self.off + nb) // 4
        self.off += nb
        self.peak = max(self.peak, self.off)
        ap = self.t[0:parts, a:b]
        if dtype != F32:
            ap = ap.bitcast(dtype)
        ap = ap[:, 0:n]
        if len(shape) == 2:
            ap = ap.rearrange("p (a b) -> p a b", a=shape[0])
        elif len(shape) == 3:
            ap = ap.rearrange("p (a b c) -> p a b c", a=shape[0], b=shape[1])
        return ap

    def mark(self):
        self.marks.append(self.off)

    def release(self):
        self.off = self.marks.pop()


D = 1024
DIN = 2000
NE = 64
SCALE = (128 + 64) ** -0.5


class _Stop(Exception):
    pass


def build_program(NSEQ=2, NG=8, CAP=384, dbg=False, phases=4):
    nc, st, P = _build_body(NSEQ, NG, CAP, dbg, phases)
    return nc


def _build_body(NSEQ, NG, CAP, dbg, phases):
    nc = bass.Bass("TRN2", target_bir_lowering=False)
    T = 1 + 4 * NG
    LP = 128 * T
    NF = 512 * NG
    NT = NSEQ * 4 * NG
    NSLOT = NE * CAP
    NB = CAP // 128

    def din(name, shape, dt=F32):
        return nc.dram_tensor(name, list(shape), dt, kind="ExternalInput").ap()
    x = din("x", [NSEQ, NF, D])
    meta = din("meta_tokens", [16, D])
    mix_norm = din("mix_norm", [1, D])
    w_in = din("w_in", [1, D, DIN])
    w_a2 = din("gla_w_a2", [1, 16, 256])
    b_a = din("gla_b_a", [1, 256])
    gla_on = din("gla_out_norm", [1, 512])
    q_norm = din("mla_q_norm", [1, 256])
    w_qb = din("mla_w_qb", [1, 256, 768])
    kv_norm = din("mla_kv_norm", [1, 128])
    w_kvb = din("mla_w_kvb", [1, 128, 1024])
    w_out = din("w_out", [1, D, D])
    ffn_norm = din("ffn_norm", [1, D])
    rgw = din("router_group_w", [1, D, 8])
    rgb = din("router_group_b", [1, 8])
    rew = din("router_expert_w", [1, D, 64])
    reb = din("router_expert_b", [1, 64])
    ewg = din("expert_w_gate", [1, NE, D, 512])
    ewu = din("expert_w_up", [1, NE, D, 512])
    ewd = din("expert_w_down", [1, NE, 512, D])
    final_norm = din("final_norm", [1, D])
    rope = din("rope_cs", [64, 2, LP])
    out = nc.dram_tensor("out", [NSEQ * NF, D], F32, kind="ExternalOutput").ap()
    skind = "ExternalOutput" if dbg else "Internal"
    mixs = nc.dram_tensor("mixs", [NSEQ * (NG + 1), 128, 8 * 512], BF16, kind=skind).ap()
    XS = nc.dram_tensor("xs_scr", [NSLOT, D], BF16, kind="Internal").ap()
    YS = nc.dram_tensor("ys_scr", [NSLOT, D], F32, kind="Internal").ap()

    st = ExitStack()
    P = Prog(nc)
    import os
    ar = Arena(nc, st, int(os.environ.get('KARENA', '205')) * 1024)
    ps = st.enter_context(nc.psum_tensor("ps", [128, 8, 512], F32))
    psb = [Buf("psb%d" % i) for i in range(8)]
    gen_rr = [0]

    def gbank():
        i = 4 + gen_rr[0] % 4
        gen_rr[0] += 1
        return ps[:, i, :], psb[i]

    def mm(out_ap, lhsT, rhs, start, stop, reads, writes):
        P.op("pe", E.matmul(out_ap, lhsT=lhsT, rhs=rhs, start=start, stop=stop), reads=reads, writes=writes)

    identf = ar.alloc([128], F32)
    identb = ar.alloc([128], BF16)
    onesb = ar.alloc([128], BF16)
    onesf = ar.alloc([128], F32)
    Ub = ar.alloc([128], BF16)
    triN = ar.alloc([128], F32)
    triUN = ar.alloc([128], F32)
    maskBD = ar.alloc([128], F32)
    padbias = ar.alloc([1], F32)
    epsb = ar.alloc([1], F32)
    tmpc = ar.alloc([128], F32)
    gates = ar.alloc([NT, 2], F32)
    slots = ar.alloc([NT, 2], I32)
    b_const = Buf("const")
    b_gates = [Buf("gates%d" % i) for i in range(NT)]

    def pool(fn, reads=(), writes=()):
        P.op("pool", fn, reads=reads, writes=writes)

    W = [b_const]
    pool(E.memset(identf, 0.0), writes=W)
    pool(E.affine_select(out=identf, in_=identf, pattern=[[-1, 128]], compare_op=ALU.not_equal,
                                   fill=1.0, base=0, channel_multiplier=1), writes=W)
    pool(E.tensor_copy(out=identb, in_=identf), writes=W)
    pool(E.memset(onesf, 1.0), writes=W)
    pool(E.memset(onesb, 1.0), writes=W)
    pool(E.memset(epsb, EPS), writes=W)
    pool(E.affine_select(out=tmpc, in_=onesf, pattern=[[1, 128]], compare_op=ALU.is_ge,
                                   fill=0.0, base=-1, channel_multiplier=-1), writes=W)
    pool(E.tensor_copy(out=Ub, in_=tmpc), writes=W)
    pool(E.affine_select(out=maskBD, in_=onesf, pattern=[[1, 128]], compare_op=ALU.is_ge,
                                   fill=0.0, base=0, channel_multiplier=-1), writes=W)
    pool(E.memset(maskBD[0:64, 64:128], 0.0), writes=W)
    pool(E.tensor_scalar(out=triN, in0=maskBD, scalar1=-1.0 / 16.0, scalar2=None, op0=ALU.mult), writes=W)
    pool(E.affine_select(out=triUN, in_=onesf, pattern=[[-1, 128]], compare_op=ALU.is_ge,
                                   fill=0.0, base=-1, channel_multiplier=1), writes=W)
    pool(E.memset(triUN[64:128, 0:64], 0.0), writes=W)
    pool(E.tensor_scalar(out=triUN, in0=triUN, scalar1=-1.0 / 16.0, scalar2=None, op0=ALU.mult), writes=W)
    pool(E.memset(padbias, -30000.0), writes=W)
    pool(E.affine_select(out=padbias, in_=padbias, pattern=[[0, 1]], compare_op=ALU.is_ge,
                                   fill=0.0, base=111, channel_multiplier=-1), writes=W)
    C = [b_const]

    def rstd_from_ss(ss_ap, n_inv, bss):
        P.op("act", E.activation(out=ss_ap, in_=ss_ap, func=AF.Sqrt, bias=epsb[0:ss_ap.shape[0], :], scale=n_inv),
             reads=[bss] + C, writes=[bss])
        P.op("dve", E.reciprocal(out=ss_ap, in_=ss_ap), reads=[bss], writes=[bss])

    ar.mark()
    NCI = DIN + 64
    w_in_b = ar.alloc([8, NCI], BF16)
    w_qb_b = ar.alloc([2, 1024], BF16)
    w_kvb_b = ar.alloc([1024], BF16)
    w_a2_b = ar.alloc([256], BF16)
    gin = ar.alloc([8], F32)
    gqn = ar.alloc([2], F32)
    gkv = ar.alloc([1], F32)
    b_w = Buf("weightsA")
    ar.mark()
    stage = ar.alloc([DIN], F32)
    b_stage = Buf("stage")
    with nc.allow_non_contiguous_dma(reason="tiny norm-gain vectors"):
        P.dma("sp", "d_w", E.dma_start(out=gin, in_=mix_norm.rearrange("o (c p) -> p (o c)", p=128)), writes=[b_w])
        P.dma("sp", "d_w", E.dma_start(out=gqn, in_=q_norm.rearrange("o (c p) -> p (o c)", p=128)), writes=[b_w])
        P.dma("sp", "d_w", E.dma_start(out=gkv, in_=kv_norm.rearrange("o (c p) -> p (o c)", p=128)), writes=[b_w])
    for c in range(8):
        P.dma("sp", "d_st", E.dma_start(out=stage, in_=w_in[0, c * 128:(c + 1) * 128, :]), writes=[b_stage])
        P.op("dve", E.tensor_scalar(out=w_in_b[:, c, 0:DIN], in0=stage, scalar1=gin[:, c:c + 1], scalar2=None, op0=ALU.mult),
             reads=[b_stage, b_w], writes=[b_w])
        P.op("dve", E.tensor_scalar(out=w_in_b[:, c, DIN:DIN + 32], in0=stage[:, 1968:2000], scalar1=gin[:, c:c + 1], scalar2=-1.0,
                                                   op0=ALU.mult, op1=ALU.mult), reads=[b_stage, b_w], writes=[b_w])
        P.op("dve", E.tensor_scalar(out=w_in_b[:, c, DIN + 32:DIN + 64], in0=stage[:, 1936:1968], scalar1=gin[:, c:c + 1], scalar2=None,
                                                   op0=ALU.mult), reads=[b_stage, b_w], writes=[b_w])
    for c in range(2):
        P.dma("sp", "d_st", E.dma_start(out=stage[:, 0:768], in_=w_qb[0, c * 128:(c + 1) * 128, :]), writes=[b_stage])
        P.op("dve", E.tensor_scalar(out=w_qb_b[:, c, 0:768], in0=stage[:, 0:768], scalar1=gqn[:, c:c + 1], scalar2=SCALE,
                                                   op0=ALU.mult, op1=ALU.mult), reads=[b_stage, b_w], writes=[b_w])
        for h in range(4):
            base = h * 192 + 128
            P.op("dve", E.tensor_scalar(out=w_qb_b[:, c, 768 + h * 64:768 + h * 64 + 32], in0=stage[:, base + 32:base + 64],
                                                                        scalar1=gqn[:, c:c + 1], scalar2=-SCALE, op0=ALU.mult, op1=ALU.mult),
                 reads=[b_stage, b_w], writes=[b_w])
            P.op("dve", E.tensor_scalar(out=w_qb_b[:, c, 768 + h * 64 + 32:768 + h * 64 + 64], in0=stage[:, base:base + 32],
                                                                        scalar1=gqn[:, c:c + 1], scalar2=SCALE, op0=ALU.mult, op1=ALU.mult),
                 reads=[b_stage, b_w], writes=[b_w])
    P.dma("sp", "d_st", E.dma_start(out=stage[:, 0:1024], in_=w_kvb[0]), writes=[b_stage])
    P.op("dve", E.tensor_scalar(out=w_kvb_b, in0=stage[:, 0:1024], scalar1=gkv[:, 0:1], scalar2=None, op0=ALU.mult),
         reads=[b_stage, b_w], writes=[b_w])
    P.dma("sp", "d_st", E.dma_start(out=stage[0:16, 0:256], in_=w_a2[0]), writes=[b_stage])
    P.dma("sp", "d_st", E.dma_start(out=stage[16:17, 0:256], in_=b_a), writes=[b_stage])
    P.op("dve", E.tensor_copy(out=w_a2_b[0:32, :], in_=stage[0:32, 0:256]), reads=[b_stage], writes=[b_w])
    ar.release()
    P.barrier()
    WA = [b_w]

    KnT = ar.alloc([4, LP], BF16)
    KrT = ar.alloc([LP], BF16)
    Vt = ar.alloc([T, 512], BF16)
    b_K = [Buf("K%d" % g) for g in range(NG + 1)]
    uT = ar.alloc([8, 512], BF16); b_uT = Buf("uT")
    mixT = uT; b_mix = b_uT
    qkT = ar.alloc([4, 512], F32); b_qk = Buf("qkT")
    silr = ar.alloc([4, 512], BF16); b_silr = Buf("silr")
    alowT = ar.alloc([512], BF16); b_alow = Buf("alowT")
    qlatT = ar.alloc([2, 512], BF16); b_qlat = Buf("qlatT")
    sqq = ar.alloc([2, 512], BF16); b_sqq = Buf("sqq")
    kvlatT = ar.alloc([512], BF16); b_kvlat = Buf("kvlatT")
    sqkv = ar.alloc([512], BF16); b_sqkv = Buf("sqkv")
    QnT = ar.alloc([4, 512], BF16); b_Qn = Buf("QnT")
    QrT = ar.alloc([4, 512], BF16); b_Qr = Buf("QrT")
    cs = ar.alloc([2, 512], F32); b_cs = Buf("cs")
    rq_bc = ar.alloc([512], F32); b_rq = Buf("rq")
    rkv_bc = ar.alloc([512], F32); b_rkv = Buf("rkv")
    rt1 = ar.alloc([512], F32); b_rt1 = Buf("rt1")
    rt2 = ar.alloc([512], F32); b_rt2 = Buf("rt2")
    xt = ar.alloc([D], F32); b_xt = Buf("xt")
    xsb = ar.alloc([D], BF16); b_xsb = Buf("xsb")
    junk = xsb; b_junk = b_xsb
    glaT = ar.alloc([4, 512], BF16); b_gla = Buf("glaT")
    ssA = ar.alloc([4], F32); b_ssA = Buf("ssA")
    ktok = ar.alloc([256], F32); b_ktok = Buf("ktok")
    vtok = ar.alloc([512], BF16); b_vtok = Buf("vtok")
    la_e = ar.alloc([256], F32); b_lae = Buf("la_e")
    la = ar.alloc([256], F32); b_la = Buf("la")
    ebT = ar.alloc([2, 128], F32); b_eb = Buf("ebT")
    enbT = ar.alloc([2, 128], F32); b_enb = Buf("enbT")
    esuf = ar.alloc([256], F32); b_esuf = Buf("esuf")
    qeT = ar.alloc([2, 128], BF16); b_qe = Buf("qeT")
    keTm = [ar.alloc([2, 128], BF16) for _ in range(2)]; b_ke = Buf("keT")
    kum = [ar.alloc([256], BF16) for _ in range(2)]; b_ku = Buf("ku")
    attm = ar.alloc([4, 128], BF16); b_attm = Buf("attm")
    Sf = ar.alloc([2, 128], F32); b_S = Buf("S")
    Sbm = [ar.alloc([2, 128], BF16) for _ in range(2)]; b_Sb = Buf("Sb")
    SbBm = [ar.alloc([2, 128], BF16) for _ in range(2)]; b_SbB = Buf("SbB")
    ost = ar.alloc([4, 128], F32); b_ost = Buf("ost")
    oT = ar.alloc([4, 128], F32); b_oT = Buf("oT")
    osq = ar.alloc([4, 128], BF16); b_osq = Buf("osq")
    orr = ar.alloc([4, 128], F32); b_orr = Buf("orr")
    PT = [ar.alloc([512], BF16) for _ in range(3)]
    b_PT = [Buf("PT%d" % i) for i in range(3)]
    PTd = [ar.alloc([512], BF16) for _ in range(4)]
    b_PTd = [Buf("PTd%d" % i) for i in range(4)]
    rinv = ar.alloc([512], F32); b_rinv = Buf("rinv")
    rkvt = ar.alloc([1], F32); b_rkvt = Buf("rkvt")
    b_mixs = [Buf("mixs%d" % i) for i in range(NSEQ * (NG + 1))]

    pool(E.memset(alowT[0:32, :], 1.0), writes=[b_alow])
    for i in range(2):
        pool(E.memset(keTm[i], 0.0), writes=[b_ke])
        pool(E.memset(kum[i], 0.0), writes=[b_ku])
        pool(E.memset(SbBm[i], 0.0), writes=[b_SbB])
    pool(E.memset(KrT, 0.0), writes=b_K)
    pool(E.memset(QrT, 0.0), writes=[b_Qr])
    for d in range(4):
        pool(E.memset(PTd[d], 0.0), writes=[b_PTd[d]])

    pt_rr = [0]
    st_rr = [0]

    for s in range(NSEQ):
        pool(E.memset(Sf, 0.0), writes=[b_S])
        for i in range(2):
            pool(E.memset(Sbm[i], 0.0), writes=[b_Sb])
        for gi in range(NG + 1):
            ntl = 1 if gi == 0 else 4
            NTOK = 128 * ntl
            tl0 = 0 if gi == 0 else 1 + 4 * (gi - 1)
            c0 = 128 * tl0
            P.dma("sp", "d_cs", E.dma_start(out=cs[0:64, :, 0:NTOK], in_=rope[:, :, c0:c0 + NTOK]), writes=[b_cs])
            for j in range(ntl):
                tl = tl0 + j
                if tl == 0:
                    pool(E.memset(xt, 0.0), writes=[b_xt])
                    P.dma("sp", "d_x", E.dma_start(out=xt[112:128, :], in_=meta), writes=[b_xt])
                else:
                    r0 = (tl - 1) * 128
                    P.dma("sp", "d_x", E.dma_start(out=xt, in_=x[s, r0:r0 + 128, :]), writes=[b_xt])
                P.op("act", E.activation(out=junk, in_=xt, func=AF.Square, accum_out=ssA[:, 0:1]), reads=[b_xt], writes=[b_junk, b_ssA])
                rstd_from_ss(ssA[:, 0:1], 1.0 / D, b_ssA)
                P.op("dve", E.tensor_scalar(out=xsb, in0=xt, scalar1=ssA[:, 0:1], scalar2=None, op0=ALU.mult),
                     reads=[b_xt, b_ssA], writes=[b_xsb])
                bk, bb = gbank()
                bkb = bk.bitcast(BF16)
                for k in range(8):
                    P.op("pe", E.transpose(out=bkb[:, k * 128:(k + 1) * 128], in_=xsb[:, k * 128:(k + 1) * 128], identity=identb),
                         reads=[b_xsb] + C, writes=[bb])
                P.op("act", E.copy(out=uT[:, :, j * 128:(j + 1) * 128], in_=bkb.rearrange("p (k t) -> p k t", k=8)),
                     reads=[bb], writes=[b_uT])

            def proj_fm(col0, M, evac):
                bk, bb = gbank()
                for k in range(8):
                    mm(bk[0:M, 0:NTOK], w_in_b[:, k, col0:col0 + M], uT[:, k, 0:NTOK], k == 0, k == 7, [b_uT] + WA, [bb])
                evac(bk[0:M, 0:NTOK], bb)
            for c in range(4):
                eng = "act" if c % 2 == 0 else "dve"
                if eng == "act":
                    proj_fm(c * 128, 128, lambda p_, bb, c=c: P.op("act", E.copy(out=qkT[:, c, 0:NTOK], in_=p_), reads=[bb], writes=[b_qk]))
                else:
                    proj_fm(c * 128, 128, lambda p_, bb, c=c: P.op("dve", E.tensor_copy(out=qkT[:, c, 0:NTOK], in_=p_), reads=[bb], writes=[b_qk]))
            for h in range(4):
                proj_fm(1024 + h * 128, 128, lambda p_, bb, h=h: P.op("act", E.activation(out=silr[:, h, 0:NTOK], in_=p_, func=AF.Silu),
                                                                         reads=[bb], writes=[b_silr]))
            proj_fm(1536, 16, lambda p_, bb: P.op("dve", E.tensor_copy(out=alowT[0:16, 0:NTOK], in_=p_), reads=[bb], writes=[b_alow]))
            for c in range(2):

                def ev(p_, bb, c=c):
                    P.op("dve", E.tensor_copy(out=qlatT[:, c, 0:NTOK], in_=p_), reads=[bb], writes=[b_qlat])
                    P.op("act", E.activation(out=sqq[:, c, 0:NTOK], in_=p_, func=AF.Square), reads=[bb], writes=[b_sqq, bb])
                proj_fm(1552 + c * 128, 128, ev)

            def evkv(p_, bb):
                P.op("dve", E.tensor_copy(out=kvlatT[:, 0:NTOK], in_=p_), reads=[bb], writes=[b_kvlat])
                P.op("act", E.activation(out=sqkv[:, 0:NTOK], in_=p_, func=AF.Square), reads=[bb], writes=[b_sqkv, bb])
            proj_fm(1808, 128, evkv)
            proj_fm(1936, 64, lambda p_, bb: P.op("dve", E.tensor_tensor(out=rt1[0:64, 0:NTOK], in0=p_, in1=cs[0:64, 0, 0:NTOK], op=ALU.mult),
                                                  reads=[bb, b_cs], writes=[b_rt1]))
            proj_fm(2000, 64, lambda p_, bb: P.op("dve", E.tensor_tensor(out=rt2[0:64, 0:NTOK], in0=p_, in1=cs[0:64, 1, 0:NTOK], op=ALU.mult),
                                                  reads=[bb, b_cs], writes=[b_rt2]))
            pool(E.tensor_tensor(out=KrT[0:64, c0:c0 + NTOK], in0=rt1[0:64, 0:NTOK], in1=rt2[0:64, 0:NTOK], op=ALU.add),
                 reads=[b_rt1, b_rt2], writes=[b_K[gi]])

            bk, bb = gbank()
            for c in range(2):
                mm(bk[:, 0:NTOK], onesb, sqq[:, c, 0:NTOK], c == 0, c == 1, [b_sqq] + C, [bb])
            P.op("act", E.activation(out=rq_bc[:, 0:NTOK], in_=bk[:, 0:NTOK], func=AF.Sqrt, bias=epsb, scale=1.0 / 256),
                 reads=[bb] + C, writes=[b_rq])
            P.op("dve", E.reciprocal(out=rq_bc[:, 0:NTOK], in_=rq_bc[:, 0:NTOK]), reads=[b_rq], writes=[b_rq])
            bk, bb = gbank()
            mm(bk[:, 0:NTOK], onesb, sqkv[:, 0:NTOK], True, True, [b_sqkv] + C, [bb])
            P.op("act", E.activation(out=rkv_bc[:, 0:NTOK], in_=bk[:, 0:NTOK], func=AF.Sqrt, bias=epsb, scale=1.0 / 128),
                 reads=[bb] + C, writes=[b_rkv])
            P.op("dve", E.reciprocal(out=rkv_bc[:, 0:NTOK], in_=rkv_bc[:, 0:NTOK]), reads=[b_rkv], writes=[b_rkv])
            for h in range(4):
                bk, bb = gbank()
                for c in range(2):
                    mm(bk[:, 0:NTOK], w_qb_b[:, c, h * 192:h * 192 + 128], qlatT[:, c, 0:NTOK], c == 0, c == 1, [b_qlat] + WA, [bb])
                P.op("dve", E.tensor_tensor(out=QnT[:, h, 0:NTOK], in0=bk[:, 0:NTOK], in1=rq_bc[:, 0:NTOK], op=ALU.mult),
                     reads=[bb, b_rq], writes=[b_Qn])
                bk, bb = gbank()
                for c in range(2):
                    mm(bk[0:64, 0:NTOK], w_qb_b[:, c, h * 192 + 128:h * 192 + 192], qlatT[:, c, 0:NTOK], c == 0, c == 1, [b_qlat] + WA, [bb])
                P.op("dve", E.tensor_tensor(out=rt1[0:64, 0:NTOK], in0=bk[0:64, 0:NTOK], in1=cs[0:64, 0, 0:NTOK], op=ALU.mult),
                     reads=[bb, b_cs], writes=[b_rt1])
                bk, bb = gbank()
                for c in range(2):
                    mm(bk[0:64, 0:NTOK], w_qb_b[:, c, 768 + h * 64:768 + h * 64 + 64], qlatT[:, c, 0:NTOK], c == 0, c == 1, [b_qlat] + WA, [bb])
                P.op("dve", E.tensor_tensor(out=rt2[0:64, 0:NTOK], in0=bk[0:64, 0:NTOK], in1=cs[0:64, 1, 0:NTOK], op=ALU.mult),
                     reads=[bb, b_cs], writes=[b_rt2])
                pool(E.tensor_tensor(out=rt1[0:64, 0:NTOK], in0=rt1[0:64, 0:NTOK], in1=rt2[0:64, 0:NTOK], op=ALU.add),
                     reads=[b_rt1, b_rt2], writes=[b_rt1])
                pool(E.tensor_tensor(out=QrT[0:64, h, 0:NTOK], in0=rt1[0:64, 0:NTOK], in1=rq_bc[0:64, 0:NTOK], op=ALU.mult),
                     reads=[b_rt1, b_rq], writes=[b_Qr])
                bk, bb = gbank()
                mm(bk[:, 0:NTOK], w_kvb_b[:, h * 256:h * 256 + 128], kvlatT[:, 0:NTOK], True, True, [b_kvlat] + WA, [bb])
                P.op("dve", E.tensor_tensor(out=KnT[:, h, c0:c0 + NTOK], in0=bk[:, 0:NTOK], in1=rkv_bc[:, 0:NTOK], op=ALU.mult),
                     reads=[bb, b_rkv], writes=[b_K[gi]])

            for j in range(ntl):
                tl = tl0 + j
                tc = slice(j * 128, (j + 1) * 128)
                bk, bb = gbank()
                mm(bk[:, 0:8], sqkv[:, tc], onesb[:, 0:8], True, True, [b_sqkv] + C, [bb])
                P.op("act", E.activation(out=rkvt, in_=bk[:, 0:1], func=AF.Sqrt, bias=epsb, scale=1.0 / 128),
                     reads=[bb] + C, writes=[b_rkvt])
                P.op("dve", E.reciprocal(out=rkvt, in_=rkvt), reads=[b_rkvt], writes=[b_rkvt])
                bk, bb = gbank()
                for h in range(4):
                    mm(bk[:, h * 128:(h + 1) * 128], kvlatT[:, tc], w_kvb_b[:, h * 256 + 128:h * 256 + 256], True, True, [b_kvlat] + WA, [bb])
                P.op("act", E.activation(out=Vt[:, tl, :], in_=bk, func=AF.Copy, scale=rkvt[:, 0:1]),
                     reads=[bb, b_rkvt], writes=[b_K[gi]])
                bk, bb = gbank()
                for k in range(8):
                    mm(bk[:, 0:256], uT[:, k, tc], w_in_b[:, k, 256:512], k == 0, k == 7, [b_uT] + WA, [bb])
                P.op("act", E.copy(out=ktok, in_=bk[:, 0:256]), reads=[bb], writes=[b_ktok])
                bk, bb = gbank()
                for k in range(8):
                    mm(bk, uT[:, k, tc], w_in_b[:, k, 512:1024], k == 0, k == 7, [b_uT] + WA, [bb])
                P.op("dve", E.tensor_copy(out=vtok, in_=bk), reads=[bb], writes=[b_vtok])
                bk, bb = gbank()
                mm(bk[:, 0:256], alowT[0:17, tc], w_a2_b[0:17, :], True, True, [b_alow] + WA, [bb])
                P.op("act", E.activation(out=la_e, in_=bk[:, 0:256], func=AF.Exp, scale=-1.0), reads=[bb], writes=[b_lae])
                P.op("act", E.activation(out=la, in_=la_e, func=AF.Ln, bias=1.0), reads=[b_lae], writes=[b_la])
                bkT, bbT = gbank()
                for pr in range(2):
                    mm(bkT[:, pr * 128:(pr + 1) * 128], la[:, pr * 128:(pr + 1) * 128], triN, True, True, [b_la] + C, [bbT])
                bkS, bbS = gbank()
                mm(bkS[:, 0:256], triUN, la, True, True, [b_la] + C, [bbS])
                P.op("act", E.activation(out=ebT, in_=bkT[:, 0:256].rearrange("p (a b) -> p a b", a=2), func=AF.Exp),
                     reads=[bbT], writes=[b_eb])
                P.op("act", E.activation(out=enbT, in_=bkT[:, 0:256].rearrange("p (a b) -> p a b", a=2), func=AF.Exp, scale=-1.0),
                     reads=[bbT], writes=[b_enb])
                P.op("act", E.activation(out=esuf, in_=bkS[:, 0:256], func=AF.Exp), reads=[bbS], writes=[b_esuf])
                P.op("dve", E.scalar_tensor_tensor(out=qeT, in0=qkT[:, 0:2, tc], scalar=0.125, in1=ebT, op0=ALU.mult, op1=ALU.mult),
                     reads=[b_qk, b_eb], writes=[b_qe])
                for i in range(2):
                    rr = slice(i * 64, i * 64 + 64)
                    P.op("dve", E.tensor_tensor(out=keTm[i][rr], in0=qkT[rr, 2:4, tc], in1=enbT[rr], op=ALU.mult), reads=[b_qk, b_enb], writes=[b_ke])
                    P.op("dve", E.tensor_tensor(out=kum[i][rr], in0=ktok[rr], in1=esuf[rr], op=ALU.mult), reads=[b_ktok, b_esuf], writes=[b_ku])
                bkA, bbA = gbank()
                for h in range(4):
                    pr, lo = h // 2, (h % 2) * 64
                    mm(bkA[:, h * 128:(h + 1) * 128], keTm[h % 2][:, pr, :], qeT[:, pr, :], True, True, [b_ke, b_qe], [bbA])
                P.op("dve", E.tensor_tensor(out=attm, in0=bkA.rearrange("p (h c) -> p h c", h=4),
                                                             in1=maskBD.unsqueeze(1).to_broadcast([128, 4, 128]), op=ALU.mult),
                     reads=[bbA] + C, writes=[b_attm])
                bkO, bbO = gbank()
                for h in range(4):
                    mm(bkO[:, h * 128:(h + 1) * 128], vtok[:, h * 128:(h + 1) * 128], attm[:, h, :], True, True, [b_vtok, b_attm], [bbO])
                bkZ, bbZ = gbank()
                for h in range(4):
                    pr, lo = h // 2, (h % 2) * 64
                    mm(bkZ[:, h * 128:h * 128 + 64], Sbm[h % 2][:, pr, :], qeT[:, pr, 0:64], True, True, [b_Sb, b_qe], [bbZ])
                for ch in range(2):
                    rows = slice(ch * 64, ch * 64 + 64)
                    bkU, bbU = gbank()
                    for h in range(4):
                        pr = h // 2
                        mm(bkU[:, h * 128:(h + 1) * 128], kum[ch][:, pr * 128:(pr + 1) * 128], vtok[:, h * 128:(h + 1) * 128], True, True,
                           [b_ku, b_vtok], [bbU])
                    col = ch * 64 + 63
                    for h in range(4):
                        pr, lo = h // 2, (h % 2) * 64
                        P.op("dve", E.scalar_tensor_tensor(
                            out=Sf[lo:lo + 64, pr, :], in0=Sf[lo:lo + 64, pr, :], scalar=ebT[lo:lo + 64, pr, col:col + 1],
                            in1=bkU[lo:lo + 64, h * 128:(h + 1) * 128], op0=ALU.mult, op1=ALU.add), reads=[bbU, b_eb, b_S], writes=[b_S])
                    if ch == 0:
                        for i in range(2):
                            pool(E.tensor_copy(out=SbBm[i][i * 64:i * 64 + 64], in_=Sf[i * 64:i * 64 + 64]), reads=[b_S], writes=[b_SbB])
                        for h in range(4):
                            pr, lo = h // 2, (h % 2) * 64
                            mm(bkZ[:, h * 128 + 64:h * 128 + 128], SbBm[h % 2][:, pr, :], qeT[:, pr, 64:128], True, True, [b_SbB, b_qe], [bbZ])
                    else:
                        for i in range(2):
                            pool(E.tensor_copy(out=Sbm[i][i * 64:i * 64 + 64], in_=Sf[i * 64:i * 64 + 64]), reads=[b_S], writes=[b_Sb])
                P.op("act", E.copy(out=ost, in_=bkZ.rearrange("p (h c) -> p h c", h=4)), reads=[bbZ], writes=[b_ost])
                P.op("dve", E.tensor_tensor(out=oT, in0=bkO.rearrange("p (h c) -> p h c", h=4), in1=ost, op=ALU.add),
                     reads=[bbO, b_ost], writes=[b_oT])
                P.op("act", E.activation(out=osq, in_=oT, func=AF.Square), reads=[b_oT], writes=[b_osq])
                bk, bb = gbank()
                mm(bk, onesb, osq.rearrange("p h c -> p (h c)"), True, True, [b_osq] + C, [bb])
                P.op("act", E.activation(out=orr, in_=bk.rearrange("p (h c) -> p h c", h=4), func=AF.Sqrt, bias=epsb, scale=1.0 / 128),
                     reads=[bb] + C, writes=[b_orr])
                P.op("dve", E.reciprocal(out=orr, in_=orr), reads=[b_orr], writes=[b_orr])
                P.op("dve", E.tensor_tensor(out=oT, in0=oT, in1=orr, op=ALU.mult), reads=[b_oT, b_orr], writes=[b_oT])
                if j == ntl - 1:
                    pass
                P.op("dve", E.tensor_tensor(out=osq, in0=oT, in1=silr[:, :, tc], op=ALU.mult), reads=[b_oT, b_silr], writes=[b_osq])
                P.op("act", E.copy(out=glaT[:, :, j * 128:(j + 1) * 128], in_=osq), reads=[b_osq], writes=[b_gla])

            nfull = 1 if gi == 0 else 4 * gi - 3
            for h in range(4 if gi > 0 else 0):
                bO, bR = ps[:, 2, :], ps[:, 3, :]
                kblocks = [(kt, None) for kt in range(nfull)]
                if gi > 0:
                    kblocks += [(tl0 + d, d) for d in range(4)]
                nkb = len(kblocks)
                for bi, (kt, d) in enumerate(kblocks):
                    kg = 0 if kt == 0 else 1 + (kt - 1) // 4
                    q0 = 0 if d is None else 128 * d
                    si = st_rr[0] % 2
                    st_rr[0] += 1
                    bS, bbS_ = ps[:, si, :], psb[si]
                    mm(bS[:, q0:NTOK], KnT[:, h, kt * 128:(kt + 1) * 128], QnT[:, h, q0:NTOK], True, False, [b_K[kg], b_Qn], [bbS_])
                    mm(bS[:, q0:NTOK], KrT[:, kt * 128:(kt + 1) * 128], QrT[:, h, q0:NTOK], False, True, [b_K[kg], b_Qr], [bbS_])
                    if d is None:
                        pi = pt_rr[0] % 3
                        pt_rr[0] += 1
                        pt, bpt = PT[pi], b_PT[pi]
                        if kt == 0:
                            P.op("act", E.activation(out=pt[:, 0:NTOK], in_=bS[:, 0:NTOK], func=AF.Exp, bias=padbias),
                                 reads=[bbS_] + C, writes=[bpt])
                        else:
                            P.op("act", E.activation(out=pt[:, 0:NTOK], in_=bS[:, 0:NTOK], func=AF.Exp), reads=[bbS_], writes=[bpt])
                    else:
                        pt, bpt = PTd[d], b_PTd[d]
                        P.op("act", E.activation(out=pt[0:64, q0:512], in_=bS[0:64, q0:512], func=AF.Exp),
                             reads=[bbS_], writes=[bpt])
                        P.op("act", E.activation(out=pt[64:128, q0 + 64:512], in_=bS[64:128, q0 + 64:512], func=AF.Exp),
                             reads=[bbS_], writes=[bpt])
                    mm(bO[:, q0:NTOK], Vt[:, kt, h * 128:(h + 1) * 128], pt[:, q0:NTOK], bi == 0, bi == nkb - 1, [b_K[kg], bpt], [psb[2]])
                    mm(bR[:, q0:NTOK], onesb, pt[:, q0:NTOK], bi == 0, bi == nkb - 1, [bpt] + C, [psb[3]])
                P.op("dve", E.reciprocal(out=rinv[:, 0:NTOK], in_=bR[:, 0:NTOK]), reads=[psb[3]], writes=[b_rinv])
                P.op("dve", E.tensor_tensor(out=mixT[:, 4 + h, 0:NTOK], in0=bO[:, 0:NTOK], in1=rinv[:, 0:NTOK], op=ALU.mult),
                     reads=[psb[2], b_rinv], writes=[b_mix])
            P.op("act", E.copy(out=mixT[:, 0:4, 0:NTOK], in_=glaT[:, :, 0:NTOK]), reads=[b_gla], writes=[b_mix])
            gidx = s * (NG + 1) + gi
            if gi > 0:
                P.dma("sp", "d_mix", E.dma_start(out=mixs[gidx], in_=mixT.rearrange("p a b -> p (a b)")), reads=[b_mix], writes=[b_mixs[gidx]])

    P.barrier()
    ar.release()

    if phases < 2:
        return _finish(nc, st, P)
    ar.mark()
    w_out_b = ar.alloc([8, D], BF16)
    wr = ar.alloc([8, 72], F32)
    rb_bc = ar.alloc([72], F32)
    g2_bc = ar.alloc([D], F32)
    gon = ar.alloc([4], F32)
    tot = ar.alloc([64], F32); b_tot = Buf("tot")
    lim = ar.alloc([64], F32)
    toti = ar.alloc([64], I32)
    b_w2 = Buf("weightsA2")
    stage2 = ar.alloc([D], F32)
    with nc.allow_non_contiguous_dma(reason="tiny norm-gain vectors / router weights"):
        P.dma("sp", "d_w", E.dma_start(out=gon, in_=gla_on.rearrange("o (c p) -> p (o c)", p=128)), writes=[b_w2])
        P.dma("sp", "d_w", E.dma_start(out=wr[:, :, 0:8], in_=rgw[0].rearrange("(c p) n -> p c n", p=128)), writes=[b_w2])
        P.dma("sp", "d_w", E.dma_start(out=wr[:, :, 8:72], in_=rew[0].rearrange("(c p) n -> p c n", p=128)), writes=[b_w2])
        P.dma("sp", "d_w", E.dma_start(out=rb_bc[:, 0:8], in_=rgb.to_broadcast([128, 8])), writes=[b_w2])
        P.dma("sp", "d_w", E.dma_start(out=rb_bc[:, 8:72], in_=reb.to_broadcast([128, 64])), writes=[b_w2])
        P.dma("sp", "d_w", E.dma_start(out=g2_bc, in_=ffn_norm.to_broadcast([128, D])), writes=[b_w2])
    for c in range(8):
        P.dma("sp", "d_st", E.dma_start(out=stage2, in_=w_out[0, c * 128:(c + 1) * 128, :]), writes=[b_stage])
        if c < 4:
            P.op("dve", E.tensor_scalar(out=w_out_b[:, c, :], in0=stage2, scalar1=gon[:, c:c + 1], scalar2=None, op0=ALU.mult),
                 reads=[b_stage, b_w2], writes=[b_w2])
        else:
            P.op("dve", E.tensor_copy(out=w_out_b[:, c, :], in_=stage2), reads=[b_stage], writes=[b_w2])
    pool(E.iota(toti, pattern=[[CAP, 64]], base=0, channel_multiplier=0), writes=[b_tot])
    pool(E.tensor_copy(out=tot, in_=toti), reads=[b_tot], writes=[b_tot])
    pool(E.tensor_scalar(out=lim, in0=tot, scalar1=float(CAP - 1), scalar2=None, op0=ALU.add), reads=[b_tot], writes=[b_w2])
    W2 = [b_w2]

    mixg = ar.alloc([8, 512], BF16); b_mixg = Buf("mixg")
    xt2 = ar.alloc([D], F32); b_xt2 = Buf("xt2")
    hT = ar.alloc([D], F32); b_h = Buf("h")
    junk2 = ar.alloc([D], F32); b_junk2 = Buf("junk2")
    u2f = ar.alloc([D], F32); b_u2f = Buf("u2f")
    u2b = [ar.alloc([D], BF16) for _ in range(2)]; b_u2b = [Buf("u2b0"), Buf("u2b1")]
    u2T = ar.alloc([8, 128], F32); b_u2T = Buf("u2T")
    ss2 = ar.alloc([1], F32); b_ss2 = Buf("ss2")
    lg = ar.alloc([72], F32); b_lg = Buf("lg")
    sm = ar.alloc([64], F32); b_sm = Buf("sm")
    t64 = ar.alloc([64], F32); b_t64 = Buf("t64")
    A1 = ar.alloc([64], F32); b_A1 = Buf("A1")
    A2 = ar.alloc([64], F32); b_A2 = Buf("A2")
    Ab = ar.alloc([64], BF16); b_Ab = Buf("Ab")
    posc = ar.alloc([64], F32); b_posc = Buf("posc")
    b_XS = Buf("XS")
    b_out = [Buf("out%d" % i) for i in range(NT)]
    zt = ar.alloc([8192], BF16); b_zt = Buf("zt")
    pool(E.memset(zt, 0.0), writes=[b_zt])
    RPP = NSLOT // 128
    XSz = XS.rearrange("(p r) d -> p (r d)", p=128)
    for c in range(RPP // 8):
        P.dma("sp", "d_z", E.dma_start(out=XSz[:, c * 8192:(c + 1) * 8192], in_=zt), reads=[b_zt], writes=[b_XS])

    for s in range(NSEQ):
        for g in range(1, NG + 1):
            gidx = s * (NG + 1) + g
            P.dma("sp", "d_mg", E.dma_start(out=mixg.rearrange("p a b -> p (a b)"), in_=mixs[gidx]), reads=[b_mixs[gidx]], writes=[b_mixg])
            for j in range(4):
                ti = s * 4 * NG + (g - 1) * 4 + j
                r0 = (g - 1) * 512 + j * 128
                tc = slice(j * 128, (j + 1) * 128)
                P.dma("sp", "d_x2", E.dma_start(out=xt2, in_=x[s, r0:r0 + 128, :]), writes=[b_xt2])
                for half in range(2):
                    bk, bb = gbank()
                    for k in range(8):
                        mm(bk, mixg[:, k, tc], w_out_b[:, k, half * 512:(half + 1) * 512], k == 0, k == 7, [b_mixg] + W2, [bb])
                    P.op("dve", E.tensor_tensor(out=hT[:, half * 512:(half + 1) * 512], in0=bk,
                                                                           in1=xt2[:, half * 512:(half + 1) * 512], op=ALU.add),
                         reads=[bb, b_xt2], writes=[b_h])
                P.dma("sp", "d_h", E.dma_start(out=out[ti * 128:(ti + 1) * 128, :], in_=hT), reads=[b_h], writes=[b_out[ti]])
                P.op("act", E.activation(out=junk2, in_=hT, func=AF.Square, accum_out=ss2), reads=[b_h], writes=[b_junk2, b_ss2])
                rstd_from_ss(ss2, 1.0 / D, b_ss2)
                P.op("dve", E.scalar_tensor_tensor(out=u2f, in0=hT, scalar=ss2[:, 0:1], in1=g2_bc, op0=ALU.mult, op1=ALU.mult),
                     reads=[b_h, b_ss2] + W2, writes=[b_u2f])
                ub, bub = u2b[ti % 2], b_u2b[ti % 2]
                P.op("act", E.copy(out=ub, in_=u2f), reads=[b_u2f], writes=[bub])
                for half in range(2):
                    bk, bb = gbank()
                    for k in range(4):
                        kk = half * 4 + k
                        P.op("pe", E.transpose(out=bk[:, k * 128:(k + 1) * 128], in_=u2f[:, kk * 128:(kk + 1) * 128], identity=identf),
                             reads=[b_u2f] + C, writes=[bb])
                    P.op("act", E.copy(out=u2T[:, half * 4:half * 4 + 4, :], in_=bk.rearrange("p (k t) -> p k t", k=4)),
                         reads=[bb], writes=[b_u2T])
                bk, bb = gbank()
                for k in range(8):
                    mm(bk[:, 0:72], u2T[:, k, :], wr[:, k, :], k == 0, k == 7, [b_u2T] + W2, [bb])
                P.op("dve", E.tensor_tensor(out=lg, in0=bk[:, 0:72], in1=rb_bc, op=ALU.add), reads=[bb] + W2, writes=[b_lg])
                gl = lg[:, 0:8]
                el3 = lg[:, 8:72].rearrange("p (g e) -> p g e", g=8)
                m8g, m8e = sm[:, 0:8], sm[:, 8:16]
                ohg, sel, oh1, oh2 = sm[:, 16:24], sm[:, 24:32], sm[:, 32:40], sm[:, 40:48]
                ngm, gsum, dd, tt = sm[:, 48:49], sm[:, 49:50], sm[:, 50:51], sm[:, 51:52]
                s1f, s2f = sm[:, 52:53], sm[:, 53:54]
                eg = sm[:, 56:64]
                R_, W_ = [b_lg, b_sm], [b_sm]

                def dv(fn, reads=R_, writes=W_):
                    P.op("dve", fn, reads=reads, writes=writes)
                dv(E.max(out=m8g, in_=gl))
                dv(E.tensor_scalar(out=ohg, in0=gl, scalar1=m8g[:, 0:1], scalar2=None, op0=ALU.is_equal))
                dv(E.tensor_scalar(out=ngm, in0=m8g[:, 0:1], scalar1=-1.0, scalar2=None, op0=ALU.mult))
                P.op("act", E.activation(out=eg, in_=gl, func=AF.Exp, bias=ngm, accum_out=gsum), reads=R_, writes=W_)
                t3 = t64.rearrange("p (g e) -> p g e", g=8)
                dv(E.tensor_tensor(out=t3, in0=el3, in1=ohg.unsqueeze(2).to_broadcast([128, 8, 8]), op=ALU.mult), writes=[b_t64])
                dv(E.tensor_reduce(out=sel, in_=t64.rearrange("p (g e) -> p e g", g=8), axis=AX.X, op=ALU.add), reads=[b_t64, b_sm])
                dv(E.max(out=m8e, in_=sel))
                dv(E.tensor_scalar(out=oh1, in0=sel, scalar1=m8e[:, 0:1], scalar2=None, op0=ALU.is_equal))
                dv(E.tensor_scalar(out=oh2, in0=sel, scalar1=m8e[:, 1:2], scalar2=None, op0=ALU.is_equal))
                dv(E.tensor_tensor(out=dd, in0=m8e[:, 1:2], in1=m8e[:, 0:1], op=ALU.subtract))
                P.op("act", E.activation(out=dd, in_=dd, func=AF.Exp), reads=R_, writes=W_)
                dv(E.tensor_scalar(out=tt, in0=dd, scalar1=1.0, scalar2=gsum, op0=ALU.add, op1=ALU.mult))
                dv(E.reciprocal(out=gates[:, ti, 0:1], in_=tt), writes=[b_sm, b_gates[ti]])
                dv(E.tensor_tensor(out=gates[:, ti, 1:2], in0=gates[:, ti, 0:1], in1=dd, op=ALU.mult), writes=[b_sm, b_gates[ti]])
                A13 = A1.rearrange("p (g e) -> p g e", g=8)
                A23 = A2.rearrange("p (g e) -> p g e", g=8)
                dv(E.tensor_tensor(out=A13, in0=ohg.unsqueeze(2).to_broadcast([128, 8, 8]), in1=oh1.unsqueeze(1).to_broadcast([128, 8, 8]), op=ALU.mult),
                   writes=[b_A1])
                dv(E.tensor_tensor(out=A23, in0=ohg.unsqueeze(2).to_broadcast([128, 8, 8]), in1=oh2.unsqueeze(1).to_broadcast([128, 8, 8]), op=ALU.mult),
                   writes=[b_A2])
                dv(E.tensor_tensor(out=Ab, in0=A1, in1=A2, op=ALU.add), reads=[b_A1, b_A2], writes=[b_Ab])
                bkc, bbc = gbank()
                mm(bkc[:, 0:64], Ub, Ab, True, True, [b_Ab] + C, [bbc])
                mm(bkc[:, 64:128], onesb, Ab, True, True, [b_Ab] + C, [bbc])
                dv(E.tensor_tensor(out=posc, in0=bkc[:, 0:64], in1=tot, op=ALU.add), reads=[bbc, b_tot], writes=[b_posc])
                dv(E.tensor_tensor(out=posc, in0=posc, in1=lim, op=ALU.min), reads=[b_posc] + W2, writes=[b_posc])
                dv(E.tensor_tensor(out=tot, in0=tot, in1=bkc[:, 64:128], op=ALU.add), reads=[bbc, b_tot, b_posc], writes=[b_tot])
                dv(E.tensor_tensor(out=t64, in0=A1, in1=posc, op=ALU.mult), reads=[b_A1, b_posc], writes=[b_t64])
                dv(E.tensor_reduce(out=s1f, in_=t64, axis=AX.X, op=ALU.add), reads=[b_t64, b_sm])
                dv(E.tensor_tensor(out=t64, in0=A2, in1=posc, op=ALU.mult), reads=[b_A2, b_posc, b_sm], writes=[b_t64])
                dv(E.tensor_reduce(out=s2f, in_=t64, axis=AX.X, op=ALU.add), reads=[b_t64, b_sm])
                dv(E.tensor_copy(out=slots[:, ti, 0:2], in_=sm[:, 52:54]), writes=[b_sm, b_gates[ti]])
                for kk in range(2):
                    P.dma("pool", "d_sc%d" % (ti % 2), E.indirect_dma_start(
                        out=XS, out_offset=bass.IndirectOffsetOnAxis(ap=slots[:, ti, kk:kk + 1], axis=0), in_=ub, in_offset=None),
                        reads=[bub, b_gates[ti]], writes=[b_XS])
    P.barrier()
    ar.release()

    if dbg:
        dg = nc.dram_tensor('dbg_gates', [128, NT * 2], F32, kind='ExternalOutput').ap()
        dsl = nc.dram_tensor('dbg_slots', [128, NT * 2], I32, kind='ExternalOutput').ap()
        P.dma('sp', 'd_dbg', E.dma_start(out=dg, in_=gates.rearrange('p a b -> p (a b)')), reads=b_gates)
        P.dma('sp', 'd_dbg', E.dma_start(out=dsl, in_=slots.rearrange('p a b -> p (a b)')), reads=b_gates)
        P.barrier()
    if phases < 3:
        return _finish(nc, st, P)
    ar.mark()
    NBUF = 2
    wst = [[ar.alloc([8, 512], F32), ar.alloc([8, 512], F32), ar.alloc([4, D], F32)] for _ in range(NBUF)]
    b_wst = [[Buf("wst%d_%d" % (i, j)) for j in range(3)] for i in range(NBUF)]
    wbf = [[ar.alloc([8, 512], BF16), ar.alloc([8, 512], BF16), ar.alloc([4, D], BF16)] for _ in range(NBUF)]
    b_wbf = [[Buf("wbf%d_%d" % (i, j)) for j in range(3)] for i in range(NBUF)]
    xsl = [ar.alloc([D], BF16) for _ in range(2)]; b_xsl = [Buf("xsl0"), Buf("xsl1")]
    XT = [ar.alloc([8, CAP], BF16) for _ in range(2)]; b_XT = [Buf("XT0"), Buf("XT1")]
    sg = [ar.alloc([CAP], F32) for _ in range(2)]; b_sg = [Buf("sg0"), Buf("sg1")]
    hTb = [ar.alloc([4, CAP], BF16) for _ in range(2)]; b_hTb = [Buf("hTb0"), Buf("hTb1")]
    ybuf = [ar.alloc([D], F32) for _ in range(2)]; b_ybuf = [Buf("ybuf0"), Buf("ybuf1")]
    b_YS = Buf("YS")
    yrr = [0]
    xrr = [0]

    def load_expert(ex):
        i = ex % NBUF
        P.dma("sp", "d_wg%d" % i, E.dma_start(out=wst[i][0], in_=ewg[0, ex].rearrange("(c p) n -> p c n", p=128)), writes=[b_wst[i][0]])
        P.dma("sp", "d_wu%d" % i, E.dma_start(out=wst[i][1], in_=ewu[0, ex].rearrange("(c p) n -> p c n", p=128)), writes=[b_wst[i][1]])
        P.dma("sp", "d_wd%d" % i, E.dma_start(out=wst[i][2], in_=ewd[0, ex].rearrange("(c p) n -> p c n", p=128)), writes=[b_wst[i][2]])

    def cast_expert(ex):
        i = ex % NBUF
        P.op("dve", E.tensor_copy(out=wbf[i][0], in_=wst[i][0]), reads=[b_wst[i][0]], writes=[b_wbf[i][0]])
        P.op("pool", E.tensor_copy(out=wbf[i][1], in_=wst[i][1]), reads=[b_wst[i][1]], writes=[b_wbf[i][1]])
        P.op("act", E.copy(out=wbf[i][2], in_=wst[i][2]), reads=[b_wst[i][2]], writes=[b_wbf[i][2]])

    load_expert(0)
    for ex in range(NE):
        i = ex % NBUF
        cast_expert(ex)
        if ex + 1 < NE:
            load_expert(ex + 1)
        xi = ex % 2
        for sb in range(NB):
            xs_i = xrr[0] % 2
            xrr[0] += 1
            r0 = ex * CAP + sb * 128
            P.dma("act", "d_xs%d" % xs_i, E.dma_start(out=xsl[xs_i], in_=XS[r0:r0 + 128, :]), reads=[b_XS], writes=[b_xsl[xs_i]])
            bk, bb = gbank()
            bkb = bk.bitcast(BF16)
            for k in range(8):
                P.op("pe", E.transpose(out=bkb[:, k * 128:(k + 1) * 128], in_=xsl[xs_i][:, k * 128:(k + 1) * 128], identity=identb),
                     reads=[b_xsl[xs_i]] + C, writes=[bb])
            P.op("dve", E.tensor_copy(out=XT[xi][:, :, sb * 128:(sb + 1) * 128], in_=bkb.rearrange("p (k t) -> p k t", k=8)),
                 reads=[bb], writes=[b_XT[xi]])
        for jh in range(4):
            hi = jh % 2
            bkG, bbG = gbank()
            for k in range(8):
                mm(bkG[:, 0:CAP], wbf[i][0][:, k, jh * 128:(jh + 1) * 128], XT[xi][:, k, :], k == 0, k == 7, [b_wbf[i][0], b_XT[xi]], [bbG])
            bkU, bbU = gbank()
            for k in range(8):
                mm(bkU[:, 0:CAP], wbf[i][1][:, k, jh * 128:(jh + 1) * 128], XT[xi][:, k, :], k == 0, k == 7, [b_wbf[i][1], b_XT[xi]], [bbU])
            P.op("act", E.activation(out=sg[hi], in_=bkG[:, 0:CAP], func=AF.Silu), reads=[bbG], writes=[b_sg[hi]])
            P.op("dve", E.tensor_tensor(out=hTb[xi][:, jh, :], in0=bkU[:, 0:CAP], in1=sg[hi], op=ALU.mult),
                 reads=[bbU, b_sg[hi]], writes=[b_hTb[xi]])
        for sb in range(NB):
            yi = yrr[0] % 2
            yrr[0] += 1
            for half in range(2):
                bk, bb = gbank()
                for jh in range(4):
                    mm(bk, hTb[xi][:, jh, sb * 128:(sb + 1) * 128], wbf[i][2][:, jh, half * 512:(half + 1) * 512], jh == 0, jh == 3,
                       [b_hTb[xi], b_wbf[i][2]], [bb])
                if half == 0:
                    P.op("act", E.copy(out=ybuf[yi][:, 0:512], in_=bk), reads=[bb], writes=[b_ybuf[yi]])
                else:
                    P.op("dve", E.tensor_copy(out=ybuf[yi][:, 512:1024], in_=bk), reads=[bb], writes=[b_ybuf[yi]])
            r0 = ex * CAP + sb * 128
            P.dma("sp", "d_ys%d" % yi, E.dma_start(out=YS[r0:r0 + 128, :], in_=ybuf[yi]), reads=[b_ybuf[yi]], writes=[b_YS])
    P.barrier()
    ar.release()

    if phases < 4:
        return _finish(nc, st, P)
    ar.mark()
    gf_bc = ar.alloc([D], F32); b_gf = Buf("gf")
    with nc.allow_non_contiguous_dma(reason="broadcast norm gain"):
        P.dma("sp", "d_w", E.dma_start(out=gf_bc, in_=final_norm.to_broadcast([128, D])), writes=[b_gf])
    hb = [ar.alloc([D], F32) for _ in range(2)]; b_hb = [Buf("hb0"), Buf("hb1")]
    y1 = [ar.alloc([D], F32) for _ in range(2)]; b_y1 = [Buf("y1_0"), Buf("y1_1")]
    y2 = [ar.alloc([D], F32) for _ in range(2)]; b_y2 = [Buf("y2_0"), Buf("y2_1")]
    ob = [ar.alloc([D], F32) for _ in range(2)]; b_ob = [Buf("ob0"), Buf("ob1")]
    junk3 = ar.alloc([D], F32); b_junk3 = Buf("junk3")
    ss3 = [ar.alloc([1], F32) for _ in range(2)]; b_ss3 = [Buf("ss3_0"), Buf("ss3_1")]
    for ti in range(NT):
        i = ti % 2
        P.dma("sp", "d_hb%d" % i, E.dma_start(out=hb[i], in_=out[ti * 128:(ti + 1) * 128, :]), reads=[b_out[ti]], writes=[b_hb[i]])
        P.dma("pool", "d_g1%d" % i, E.indirect_dma_start(
            out=y1[i], out_offset=None, in_=YS, in_offset=bass.IndirectOffsetOnAxis(ap=slots[:, ti, 0:1], axis=0)), reads=[b_YS, b_gates[ti]], writes=[b_y1[i]])
        P.dma("pool", "d_g2%d" % i, E.indirect_dma_start(
            out=y2[i], out_offset=None, in_=YS, in_offset=bass.IndirectOffsetOnAxis(ap=slots[:, ti, 1:2], axis=0)), reads=[b_YS, b_gates[ti]], writes=[b_y2[i]])
        P.op("dve", E.scalar_tensor_tensor(out=hb[i], in0=y1[i], scalar=gates[:, ti, 0:1], in1=hb[i], op0=ALU.mult, op1=ALU.add),
             reads=[b_y1[i], b_hb[i], b_gates[ti]], writes=[b_hb[i]])
        P.op("dve", E.scalar_tensor_tensor(out=hb[i], in0=y2[i], scalar=gates[:, ti, 1:2], in1=hb[i], op0=ALU.mult, op1=ALU.add),
             reads=[b_y2[i], b_hb[i], b_gates[ti]], writes=[b_hb[i]])
        P.op("act", E.activation(out=junk3, in_=hb[i], func=AF.Square, accum_out=ss3[i]), reads=[b_hb[i]], writes=[b_junk3, b_ss3[i]])
        rstd_from_ss(ss3[i], 1.0 / D, b_ss3[i])
        P.op("dve", E.scalar_tensor_tensor(out=ob[i], in0=hb[i], scalar=ss3[i][:, 0:1], in1=gf_bc, op0=ALU.mult, op1=ALU.mult),
             reads=[b_hb[i], b_ss3[i], b_gf], writes=[b_ob[i]])
        P.dma("sp", "d_o%d" % i, E.dma_start(out=out[ti * 128:(ti + 1) * 128, :], in_=ob[i]), reads=[b_ob[i], b_hb[i]], writes=[b_out[ti]])
    P.barrier()
    ar.release()
    return _finish(nc, st, P)


def _finish(nc, st, P):
    P.barrier()
    P.emit(st)
    st.close()
    return nc, st, P


def rope_table(NG):
    T = 1 + 4 * NG
    LP = 128 * T
    pos = (np.arange(LP, dtype=np.float32) - np.float32(112.0)).astype(np.float32)
    inv = (np.float32(10000.0) ** (-np.arange(0, 64, 2, dtype=np.float32) / np.float32(64))).astype(np.float32)
    ang = (pos[None, :] * inv[:, None]).astype(np.float32)
    cs = np.zeros((64, 2, LP), np.float32)
    cs[0:32, 0] = np.cos(ang); cs[32:64, 0] = np.cos(ang)
    cs[0:32, 1] = np.sin(ang); cs[32:64, 1] = np.sin(ang)
    return cs


_CACHE = {}


def run(inputs, n_cores, NSEQ, NG, CAP, dbg=False, phases=4):
    key = (NSEQ, NG, CAP, dbg, phases)
    if key not in _CACHE:
        _CACHE[key] = build_program(NSEQ, NG, CAP, dbg, phases)
    nc = _CACHE[key]
    x = np.ascontiguousarray(inputs["x"], dtype=np.float32)
    cs = rope_table(NG)
    in_maps = []
    for c in range(n_cores):
        m = {"x": x[c * NSEQ:(c + 1) * NSEQ], "rope_cs": cs}
        for k, v in inputs.items():
            if k == "x":
                continue
            a = np.ascontiguousarray(v, dtype=np.float32)
            if k == "final_norm":
                a = a.reshape(1, -1)
            m[k] = a
        in_maps.append(m)
    res = run_bass_kernel_spmd(nc, in_maps, core_ids=list(range(n_cores)))
    return res


def kernel(**inputs):
    x = inputs["x"]
    B, S, Dm = x.shape
    res = run(inputs, 8, 2, 8, 384)
    outs = [r["out"].reshape(2, S, Dm) for r in res.results]
    return np.concatenate(outs, axis=0).astype(np.float32)
```

```python
import itertools
import numpy as np
from contextlib import ExitStack
import concourse.bass as bass
import concourse.mybir as mybir
from concourse.bass_utils import run_bass_kernel_spmd

F32 = mybir.dt.float32
BF16 = mybir.dt.bfloat16
I32 = mybir.dt.int32
AF = mybir.ActivationFunctionType
ALU = mybir.AluOpType
AX = mybir.AxisListType
ENGS = ("pe", "act", "dve", "pool", "sp")
EPS = 1e-6


class Buf:
    __slots__ = ("name", "lw", "rd")

    def __init__(self, name):
        self.name = name
        self.lw = None
        self.rd = {}


class Call:
    __slots__ = ("name", "args", "kwargs")

    def __init__(self, name, args, kwargs):
        self.name, self.args, self.kwargs = name, args, kwargs


class _Rec:
    def __getattr__(self, name):
        def f(*args, **kwargs):
            return Call(name, args, kwargs)
        return f


E = _Rec()


class Prog:
    def __init__(self, nc):
        self.nc = nc
        self.ops = {e: [] for e in ENGS}
        self.cnt = {e: 0 for e in ENGS}
        self.seen = {e: {} for e in ENGS}
        self.sems = {}
        self.dcnt = {}
        import os
        self.limit = int(os.environ.get("KLIMIT", "100000000"))

    def _deps(self, eng, reads, writes):
        ev = {}

        def need(e):
            if e is None:
                return
            k, v = e
            if ev.get(k, 0) < v:
                ev[k] = v
        for b in reads:
            need(b.lw)
        for b in writes:
            need(b.lw)
            for k, v in b.rd.items():
                need((k, v))
        waits = []
        for k, v in ev.items():
            if k == eng and eng in ("pe", "sp"):
                continue
            if self.seen[eng].get(k, 0) >= v:
                continue
            self.seen[eng][k] = v
            waits.append((k, v))
        return waits

    def _commit(self, event, reads, writes):
        k, v = event
        for b in reads:
            if b.rd.get(k, 0) < v:
                b.rd[k] = v
        for b in writes:
            b.lw = event
            b.rd = {}

    def op(self, eng, fn, reads=(), writes=()):
        self.nrec = getattr(self, "nrec", 0) + 1
        if self.nrec > self.limit:
            return
        waits = self._deps(eng, reads, writes)
        self.cnt[eng] += 1
        self.ops[eng].append((waits, fn, (eng, 1)))
        self._commit((eng, self.cnt[eng]), reads, writes)

    def dma(self, eng, dsem, fn, reads=(), writes=()):
        self.nrec = getattr(self, "nrec", 0) + 1
        if self.nrec > self.limit:
            return
        waits = self._deps(eng, reads, writes)
        self.dcnt[dsem] = self.dcnt.get(dsem, 0) + 16
        self.ops[eng].append((waits, fn, (dsem, 16)))
        self._commit((dsem, self.dcnt[dsem]), reads, writes)

    def barrier(self):
        for e in ENGS:
            waits = []
            for k in ENGS:
                if k != e and self.cnt[k] > self.seen[e].get(k, 0):
                    self.seen[e][k] = self.cnt[k]
                    waits.append((k, self.cnt[k]))
            for k, v in self.dcnt.items():
                if v > self.seen[e].get(k, 0):
                    self.seen[e][k] = v
                    waits.append((k, v))
            self.ops[e].append((waits, None, None))

    def emit(self, stack):
        nc = self.nc
        keys = set(ENGS) | set(self.dcnt.keys())
        for k in sorted(keys):
            self.sems[k] = stack.enter_context(nc.semaphore("s_" + k))
        stack.enter_context(nc.allow_non_contiguous_dma(reason="tiny gain vectors / router weights / broadcasts"))
        block = stack.enter_context(nc.Block())
        sems = self.sems

        def runner(e):
            lst = self.ops[e]

            def body(engobj):
                for waits, fn, inc in lst:
                    for k, v in waits:
                        engobj.wait_ge(sems[k], v)
                    if fn is not None:
                        getattr(engobj, fn.name)(*fn.args, **fn.kwargs).then_inc(sems[inc[0]], inc[1])
            return body
        block.tensor(runner("pe"))
        block.scalar(runner("act"))
        block.vector(runner("dve"))
        block.gpsimd(runner("pool"))
        block.sync(runner("sp"))


class Arena:
    def __init__(self, nc, stack, nbytes):
        self.t = stack.enter_context(nc.sbuf_tensor("arena", [128, nbytes // 4], F32))
        self.nbytes = nbytes
        self.off = 0
        self.marks = []
        self.peak = 0

    def alloc(self, shape, dtype, parts=128):
        esz = 2 if dtype == BF16 else 4
        n = int(np.prod(shape))
        nb = (n * esz + 63) // 64 * 64
        assert self.off + nb <= self.nbytes, f"arena overflow {self.off}+{nb}>{self.nbytes}"
        a, b = self.off // 4, (self.off + nb) // 4
        self.off += nb
        self.peak = max(self.peak, self.off)
        ap = self.t[0:parts, a:b]
        if dtype != F32:
            ap = ap.bitcast(dtype)
        ap = ap[:, 0:n]
        if len(shape) == 2:
            ap = ap.rearrange("p (a b) -> p a b", a=shape[0])
        elif len(shape) == 3:
            ap = ap.rearrange("p (a b c) -> p a b c", a=shape[0], b=shape[1])
        return ap

    def mark(self):
        self.marks.append(self.off)

    def release(self):
        self.off = self.marks.pop()


D = 1024
DIN = 2000
NE = 64
SCALE = (128 + 64) ** -0.5


class _Stop(Exception):
    pass


def build_program(NSEQ=2, NG=8, CAP=384, dbg=False, phases=4):
    nc, st, P = _build_body(NSEQ, NG, CAP, dbg, phases)
    return nc


def _build_body(NSEQ, NG, CAP, dbg, phases):
    nc = bass.Bass("TRN2", target_bir_lowering=False)
    T = 1 + 4 * NG
    LP = 128 * T
    NF = 512 * NG
    NT = NSEQ * 4 * NG
    NSLOT = NE * CAP
    NB = CAP // 128

    def din(name, shape, dt=F32):
        return nc.dram_tensor(name, list(shape), dt, kind="ExternalInput").ap()
    x = din("x", [NSEQ, NF, D])
    meta = din("meta_tokens", [16, D])
    mix_norm = din("mix_norm", [1, D])
    w_in = din("w_in", [1, D, DIN])
    w_a2 = din("gla_w_a2", [1, 16, 256])
    b_a = din("gla_b_a", [1, 256])
    gla_on = din("gla_out_norm", [1, 512])
    q_norm = din("mla_q_norm", [1, 256])
    w_qb = din("mla_w_qb", [1, 256, 768])
    kv_norm = din("mla_kv_norm", [1, 128])
    w_kvb = din("mla_w_kvb", [1, 128, 1024])
    w_out = din("w_out", [1, D, D])
    ffn_norm = din("ffn_norm", [1, D])
    rgw = din("router_group_w", [1, D, 8])
    rgb = din("router_group_b", [1, 8])
    rew = din("router_expert_w", [1, D, 64])
    reb = din("router_expert_b", [1, 64])
    ewg = din("expert_w_gate", [1, NE, D, 512])
    ewu = din("expert_w_up", [1, NE, D, 512])
    ewd = din("expert_w_down", [1, NE, 512, D])
    final_norm = din("final_norm", [1, D])
    rope = din("rope_cs", [64, 2, LP])
    out = nc.dram_tensor("out", [NSEQ * NF, D], F32, kind="ExternalOutput").ap()
    skind = "ExternalOutput" if dbg else "Internal"
    mixs = nc.dram_tensor("mixs", [NSEQ * (NG + 1), 128, 8 * 512], BF16, kind=skind).ap()
    XS = nc.dram_tensor("xs_scr", [NSLOT, D], BF16, kind="Internal").ap()
    YS = nc.dram_tensor("ys_scr", [NSLOT, D], F32, kind="Internal").ap()

    st = ExitStack()
    P = Prog(nc)
    import os
    ar = Arena(nc, st, int(os.environ.get('KARENA', '205')) * 1024)
    ps = st.enter_context(nc.psum_tensor("ps", [128, 8, 512], F32))
    psb = [Buf("psb%d" % i) for i in range(8)]
    gen_rr = [0]

    def gbank():
        i = 4 + gen_rr[0] % 4
        gen_rr[0] += 1
        return ps[:, i, :], psb[i]

    def mm(out_ap, lhsT, rhs, start, stop, reads, writes):
        P.op("pe", E.matmul(out_ap, lhsT=lhsT, rhs=rhs, start=start, stop=stop), reads=reads, writes=writes)

    identf = ar.alloc([128], F32)
    identb = ar.alloc([128], BF16)
    onesb = ar.alloc([128], BF16)
    onesf = ar.alloc([128], F32)
    Ub = ar.alloc([128], BF16)
    triN = ar.alloc([128], F32)
    triUN = ar.alloc([128], F32)
    maskBD = ar.alloc([128], F32)
    padbias = ar.alloc([1], F32)
    epsb = ar.alloc([1], F32)
    tmpc = ar.alloc([128], F32)
    gates = ar.alloc([NT, 2], F32)
    slots = ar.alloc([NT, 2], I32)
    b_const = Buf("const")
    b_gates = [Buf("gates%d" % i) for i in range(NT)]

    def pool(fn, reads=(), writes=()):
        P.op("pool", fn, reads=reads, writes=writes)

    W = [b_const]
    pool(E.memset(identf, 0.0), writes=W)
    pool(E.affine_select(out=identf, in_=identf, pattern=[[-1, 128]], compare_op=ALU.not_equal,
                                   fill=1.0, base=0, channel_multiplier=1), writes=W)
    pool(E.tensor_copy(out=identb, in_=identf), writes=W)
    pool(E.memset(onesf, 1.0), writes=W)
    pool(E.memset(onesb, 1.0), writes=W)
    pool(E.memset(epsb, EPS), writes=W)
    pool(E.affine_select(out=tmpc, in_=onesf, pattern=[[1, 128]], compare_op=ALU.is_ge,
                                   fill=0.0, base=-1, channel_multiplier=-1), writes=W)
    pool(E.tensor_copy(out=Ub, in_=tmpc), writes=W)
    pool(E.affine_select(out=maskBD, in_=onesf, pattern=[[1, 128]], compare_op=ALU.is_ge,
                                   fill=0.0, base=0, channel_multiplier=-1), writes=W)
    pool(E.memset(maskBD[0:64, 64:128], 0.0), writes=W)
    pool(E.tensor_scalar(out=triN, in0=maskBD, scalar1=-1.0 / 16.0, scalar2=None, op0=ALU.mult), writes=W)
    pool(E.affine_select(out=triUN, in_=onesf, pattern=[[-1, 128]], compare_op=ALU.is_ge,
                                   fill=0.0, base=-1, channel_multiplier=1), writes=W)
    pool(E.memset(triUN[64:128, 0:64], 0.0), writes=W)
    pool(E.tensor_scalar(out=triUN, in0=triUN, scalar1=-1.0 / 16.0, scalar2=None, op0=ALU.mult), writes=W)
    pool(E.memset(padbias, -30000.0), writes=W)
    pool(E.affine_select(out=padbias, in_=padbias, pattern=[[0, 1]], compare_op=ALU.is_ge,
                                   fill=0.0, base=111, channel_multiplier=-1), writes=W)
    C = [b_const]

    def rstd_from_ss(ss_ap, n_inv, bss):
        P.op("act", E.activation(out=ss_ap, in_=ss_ap, func=AF.Sqrt, bias=epsb[0:ss_ap.shape[0], :], scale=n_inv),
             reads=[bss] + C, writes=[bss])
        P.op("dve", E.reciprocal(out=ss_ap, in_=ss_ap), reads=[bss], writes=[bss])

    ar.mark()
    NCI = DIN + 64
    w_in_b = ar.alloc([8, NCI], BF16)
    w_qb_b = ar.alloc([2, 1024], BF16)
    w_kvb_b = ar.alloc([1024], BF16)
    w_a2_b = ar.alloc([256], BF16)
    gin = ar.alloc([8], F32)
    gqn = ar.alloc([2], F32)
    gkv = ar.alloc([1], F32)
    b_w = Buf("weightsA")
    ar.mark()
    stage = ar.alloc([DIN], F32)
    b_stage = Buf("stage")
    with nc.allow_non_contiguous_dma(reason="tiny norm-gain vectors"):
        P.dma("sp", "d_w", E.dma_start(out=gin, in_=mix_norm.rearrange("o (c p) -> p (o c)", p=128)), writes=[b_w])
        P.dma("sp", "d_w", E.dma_start(out=gqn, in_=q_norm.rearrange("o (c p) -> p (o c)", p=128)), writes=[b_w])
        P.dma("sp", "d_w", E.dma_start(out=gkv, in_=kv_norm.rearrange("o (c p) -> p (o c)", p=128)), writes=[b_w])
    for c in range(8):
        P.dma("sp", "d_st", E.dma_start(out=stage, in_=w_in[0, c * 128:(c + 1) * 128, :]), writes=[b_stage])
        P.op("dve", E.tensor_scalar(out=w_in_b[:, c, 0:DIN], in0=stage, scalar1=gin[:, c:c + 1], scalar2=None, op0=ALU.mult),
             reads=[b_stage, b_w], writes=[b_w])
        P.op("dve", E.tensor_scalar(out=w_in_b[:, c, DIN:DIN + 32], in0=stage[:, 1968:2000], scalar1=gin[:, c:c + 1], scalar2=-1.0,
                                                   op0=ALU.mult, op1=ALU.mult), reads=[b_stage, b_w], writes=[b_w])
        P.op("dve", E.tensor_scalar(out=w_in_b[:, c, DIN + 32:DIN + 64], in0=stage[:, 1936:1968], scalar1=gin[:, c:c + 1], scalar2=None,
                                                   op0=ALU.mult), reads=[b_stage, b_w], writes=[b_w])
    for c in range(2):
        P.dma("sp", "d_st", E.dma_start(out=stage[:, 0:768], in_=w_qb[0, c * 128:(c + 1) * 128, :]), writes=[b_stage])
        P.op("dve", E.tensor_scalar(out=w_qb_b[:, c, 0:768], in0=stage[:, 0:768], scalar1=gqn[:, c:c + 1], scalar2=SCALE,
                                                   op0=ALU.mult, op1=ALU.mult), reads=[b_stage, b_w], writes=[b_w])
        for h in range(4):
            base = h * 192 + 128
            P.op("dve", E.tensor_scalar(out=w_qb_b[:, c, 768 + h * 64:768 + h * 64 + 32], in0=stage[:, base + 32:base + 64],
                                                                        scalar1=gqn[:, c:c + 1], scalar2=-SCALE, op0=ALU.mult, op1=ALU.mult),
                 reads=[b_stage, b_w], writes=[b_w])
            P.op("dve", E.tensor_scalar(out=w_qb_b[:, c, 768 + h * 64 + 32:768 + h * 64 + 64], in0=stage[:, base:base + 32],
                                                                        scalar1=gqn[:, c:c + 1], scalar2=SCALE, op0=ALU.mult, op1=ALU.mult),
                 reads=[b_stage, b_w], writes=[b_w])
    P.dma("sp", "d_st", E.dma_start(out=stage[:, 0:1024], in_=w_kvb[0]), writes=[b_stage])
    P.op("dve", E.tensor_scalar(out=w_kvb_b, in0=stage[:, 0:1024], scalar1=gkv[:, 0:1], scalar2=None, op0=ALU.mult),
         reads=[b_stage, b_w], writes=[b_w])
    P.dma("sp", "d_st", E.dma_start(out=stage[0:16, 0:256], in_=w_a2[0]), writes=[b_stage])
    P.dma("sp", "d_st", E.dma_start(out=stage[16:17, 0:256], in_=b_a), writes=[b_stage])
    P.op("dve", E.tensor_copy(out=w_a2_b[0:32, :], in_=stage[0:32, 0:256]), reads=[b_stage], writes=[b_w])
    ar.release()
    P.barrier()
    WA = [b_w]

    KnT = ar.alloc([4, LP], BF16)
    KrT = ar.alloc([LP], BF16)
    Vt = ar.alloc([T, 512], BF16)
    b_K = [Buf("K%d" % g) for g in range(NG + 1)]
    uT = ar.alloc([8, 512], BF16); b_uT = Buf("uT")
    mixT = ar.alloc([8, 512], BF16); b_mix = Buf("mixT")
    qkT = ar.alloc([4, 512], F32); b_qk = Buf("qkT")
    silr = ar.alloc([4, 512], BF16); b_silr = Buf("silr")
    alowT = ar.alloc([512], BF16); b_alow = Buf("alowT")
    qlatT = ar.alloc([2, 512], BF16); b_qlat = Buf("qlatT")
    sqq = ar.alloc([2, 512], BF16); b_sqq = Buf("sqq")
    kvlatT = ar.alloc([512], BF16); b_kvlat = Buf("kvlatT")
    sqkv = ar.alloc([512], BF16); b_sqkv = Buf("sqkv")
    QnT = ar.alloc([4, 512], BF16); b_Qn = Buf("QnT")
    QrT = ar.alloc([4, 512], BF16); b_Qr = Buf("QrT")
    cs = ar.alloc([2, 512], F32); b_cs = Buf("cs")
    rq_bc = ar.alloc([512], F32); b_rq = Buf("rq")
    rkv_bc = ar.alloc([512], F32); b_rkv = Buf("rkv")
    rt1 = ar.alloc([512], F32); b_rt1 = Buf("rt1")
    rt2 = ar.alloc([512], F32); b_rt2 = Buf("rt2")
    xt = ar.alloc([D], F32); b_xt = Buf("xt")
    xsb = ar.alloc([D], BF16); b_xsb = Buf("xsb")
    junk = xsb; b_junk = b_xsb
    b_gla = Buf("mixT_gla")
    ssA = ar.alloc([4], F32); b_ssA = Buf("ssA")
    ktok = ar.alloc([256], F32); b_ktok = Buf("ktok")
    vtok = ar.alloc([512], BF16); b_vtok = Buf("vtok")
    la_e = ar.alloc([256], F32); b_lae = Buf("la_e")
    la = ar.alloc([256], F32); b_la = Buf("la")
    ebT = ar.alloc([2, 128], F32); b_eb = Buf("ebT")
    enbT = ar.alloc([2, 128], F32); b_enb = Buf("enbT")
    esuf = ar.alloc([256], F32); b_esuf = Buf("esuf")
    qeT = ar.alloc([2, 128], BF16); b_qe = Buf("qeT")
    keTm = [ar.alloc([2, 128], BF16) for _ in range(2)]; b_ke = Buf("keT")
    kum = [ar.alloc([256], BF16) for _ in range(2)]; b_ku = Buf("ku")
    attm = ar.alloc([4, 128], BF16); b_attm = Buf("attm")
    Sf = ar.alloc([2, 128], F32); b_S = Buf("S")
    Sbm = [ar.alloc([2, 128], BF16) for _ in range(2)]; b_Sb = Buf("Sb")
    SbBm = [ar.alloc([2, 128], BF16) for _ in range(2)]; b_SbB = Buf("SbB")
    ost = ar.alloc([4, 128], F32); b_ost = Buf("ost")
    oT = ar.alloc([4, 128], F32); b_oT = Buf("oT")
    osq = ar.alloc([4, 128], BF16); b_osq = Buf("osq")
    orr = rt1.rearrange("p (a b) -> p a b", a=4); b_orr = b_rt1
    PT = [ar.alloc([512], BF16) for _ in range(3)]
    b_PT = [Buf("PT%d" % i) for i in range(3)]
    PTd = [ar.alloc([512], BF16) for _ in range(4)]
    b_PTd = [Buf("PTd%d" % i) for i in range(4)]
    rinv = rt2; b_rinv = b_rt2
    rkvt = ar.alloc([1], F32); b_rkvt = Buf("rkvt")
    b_mixs = [Buf("mixs%d" % i) for i in range(NSEQ * (NG + 1))]

    pool(E.memset(alowT[0:32, :], 1.0), writes=[b_alow])
    for i in range(2):
        pool(E.memset(keTm[i], 0.0), writes=[b_ke])
        pool(E.memset(kum[i], 0.0), writes=[b_ku])
        pool(E.memset(SbBm[i], 0.0), writes=[b_SbB])
    pool(E.memset(KrT, 0.0), writes=b_K)
    pool(E.memset(QrT, 0.0), writes=[b_Qr])
    for d in range(4):
        pool(E.memset(PTd[d], 0.0), writes=[b_PTd[d]])

    pt_rr = [0]
    st_rr = [0]

    for s in range(NSEQ):
        pool(E.memset(Sf, 0.0), writes=[b_S])
        for i in range(2):
            pool(E.memset(Sbm[i], 0.0), writes=[b_Sb])
        for gi in range(NG + 1):
            ntl = 1 if gi == 0 else 4
            NTOK = 128 * ntl
            tl0 = 0 if gi == 0 else 1 + 4 * (gi - 1)
            c0 = 128 * tl0
            P.dma("sp", "d_cs", E.dma_start(out=cs[0:64, :, 0:NTOK], in_=rope[:, :, c0:c0 + NTOK]), writes=[b_cs])
            for j in range(ntl):
                tl = tl0 + j
                if tl == 0:
                    pool(E.memset(xt, 0.0), writes=[b_xt])
                    P.dma("sp", "d_x", E.dma_start(out=xt[112:128, :], in_=meta), writes=[b_xt])
                else:
                    r0 = (tl - 1) * 128
                    P.dma("sp", "d_x", E.dma_start(out=xt, in_=x[s, r0:r0 + 128, :]), writes=[b_xt])
                P.op("act", E.activation(out=junk, in_=xt, func=AF.Square, accum_out=ssA[:, 0:1]), reads=[b_xt], writes=[b_junk, b_ssA])
                rstd_from_ss(ssA[:, 0:1], 1.0 / D, b_ssA)
                P.op("dve", E.tensor_scalar(out=xsb, in0=xt, scalar1=ssA[:, 0:1], scalar2=None, op0=ALU.mult),
                     reads=[b_xt, b_ssA], writes=[b_xsb])
                bk, bb = gbank()
                bkb = bk.bitcast(BF16)
                for k in range(8):
                    P.op("pe", E.transpose(out=bkb[:, k * 128:(k + 1) * 128], in_=xsb[:, k * 128:(k + 1) * 128], identity=identb),
                         reads=[b_xsb] + C, writes=[bb])
                P.op("act", E.copy(out=uT[:, :, j * 128:(j + 1) * 128], in_=bkb.rearrange("p (k t) -> p k t", k=8)),
                     reads=[bb], writes=[b_uT])

            def proj_fm(col0, M, evac):
                bk, bb = gbank()
                for k in range(8):
                    mm(bk[0:M, 0:NTOK], w_in_b[:, k, col0:col0 + M], uT[:, k, 0:NTOK], k == 0, k == 7, [b_uT] + WA, [bb])
                evac(bk[0:M, 0:NTOK], bb)
            for c in range(4):
                eng = "act" if c % 2 == 0 else "dve"
                if eng == "act":
                    proj_fm(c * 128, 128, lambda p_, bb, c=c: P.op("act", E.copy(out=qkT[:, c, 0:NTOK], in_=p_), reads=[bb], writes=[b_qk]))
                else:
                    proj_fm(c * 128, 128, lambda p_, bb, c=c: P.op("dve", E.tensor_copy(out=qkT[:, c, 0:NTOK], in_=p_), reads=[bb], writes=[b_qk]))
            for h in range(4):
                proj_fm(1024 + h * 128, 128, lambda p_, bb, h=h: P.op("act", E.activation(out=silr[:, h, 0:NTOK], in_=p_, func=AF.Silu),
                                                                         reads=[bb], writes=[b_silr]))
            proj_fm(1536, 16, lambda p_, bb: P.op("dve", E.tensor_copy(out=alowT[0:16, 0:NTOK], in_=p_), reads=[bb], writes=[b_alow]))
            for c in range(2):

                def ev(p_, bb, c=c):
                    P.op("dve", E.tensor_copy(out=qlatT[:, c, 0:NTOK], in_=p_), reads=[bb], writes=[b_qlat])
                    P.op("act", E.activation(out=sqq[:, c, 0:NTOK], in_=p_, func=AF.Square), reads=[bb], writes=[b_sqq, bb])
                proj_fm(1552 + c * 128, 128, ev)

            def evkv(p_, bb):
                P.op("dve", E.tensor_copy(out=kvlatT[:, 0:NTOK], in_=p_), reads=[bb], writes=[b_kvlat])
                P.op("act", E.activation(out=sqkv[:, 0:NTOK], in_=p_, func=AF.Square), reads=[bb], writes=[b_sqkv, bb])
            proj_fm(1808, 128, evkv)
            proj_fm(1936, 64, lambda p_, bb: P.op("dve", E.tensor_tensor(out=rt1[0:64, 0:NTOK], in0=p_, in1=cs[0:64, 0, 0:NTOK], op=ALU.mult),
                                                  reads=[bb, b_cs], writes=[b_rt1]))
            proj_fm(2000, 64, lambda p_, bb: P.op("dve", E.tensor_tensor(out=rt2[0:64, 0:NTOK], in0=p_, in1=cs[0:64, 1, 0:NTOK], op=ALU.mult),
                                                  reads=[bb, b_cs], writes=[b_rt2]))
            pool(E.tensor_tensor(out=KrT[0:64, c0:c0 + NTOK], in0=rt1[0:64, 0:NTOK], in1=rt2[0:64, 0:NTOK], op=ALU.add),
                 reads=[b_rt1, b_rt2], writes=[b_K[gi]])

            bk, bb = gbank()
            for c in range(2):
                mm(bk[:, 0:NTOK], onesb, sqq[:, c, 0:NTOK], c == 0, c == 1, [b_sqq] + C, [bb])
            P.op("act", E.activation(out=rq_bc[:, 0:NTOK], in_=bk[:, 0:NTOK], func=AF.Sqrt, bias=epsb, scale=1.0 / 256),
                 reads=[bb] + C, writes=[b_rq])
            P.op("dve", E.reciprocal(out=rq_bc[:, 0:NTOK], in_=rq_bc[:, 0:NTOK]), reads=[b_rq], writes=[b_rq])
            bk, bb = gbank()
            mm(bk[:, 0:NTOK], onesb, sqkv[:, 0:NTOK], True, True, [b_sqkv] + C, [bb])
            P.op("act", E.activation(out=rkv_bc[:, 0:NTOK], in_=bk[:, 0:NTOK], func=AF.Sqrt, bias=epsb, scale=1.0 / 128),
                 reads=[bb] + C, writes=[b_rkv])
            P.op("dve", E.reciprocal(out=rkv_bc[:, 0:NTOK], in_=rkv_bc[:, 0:NTOK]), reads=[b_rkv], writes=[b_rkv])
            for h in range(4):
                bk, bb = gbank()
                for c in range(2):
                    mm(bk[:, 0:NTOK], w_qb_b[:, c, h * 192:h * 192 + 128], qlatT[:, c, 0:NTOK], c == 0, c == 1, [b_qlat] + WA, [bb])
                P.op("dve", E.tensor_tensor(out=QnT[:, h, 0:NTOK], in0=bk[:, 0:NTOK], in1=rq_bc[:, 0:NTOK], op=ALU.mult),
                     reads=[bb, b_rq], writes=[b_Qn])
                bk, bb = gbank()
                for c in range(2):
                    mm(bk[0:64, 0:NTOK], w_qb_b[:, c, h * 192 + 128:h * 192 + 192], qlatT[:, c, 0:NTOK], c == 0, c == 1, [b_qlat] + WA, [bb])
                P.op("dve", E.tensor_tensor(out=rt1[0:64, 0:NTOK], in0=bk[0:64, 0:NTOK], in1=cs[0:64, 0, 0:NTOK], op=ALU.mult),
                     reads=[bb, b_cs], writes=[b_rt1])
                bk, bb = gbank()
                for c in range(2):
                    mm(bk[0:64, 0:NTOK], w_qb_b[:, c, 768 + h * 64:768 + h * 64 + 64], qlatT[:, c, 0:NTOK], c == 0, c == 1, [b_qlat] + WA, [bb])
                P.op("dve", E.tensor_tensor(out=rt2[0:64, 0:NTOK], in0=bk[0:64, 0:NTOK], in1=cs[0:64, 1, 0:NTOK], op=ALU.mult),
                     reads=[bb, b_cs], writes=[b_rt2])
                pool(E.tensor_tensor(out=rt1[0:64, 0:NTOK], in0=rt1[0:64, 0:NTOK], in1=rt2[0:64, 0:NTOK], op=ALU.add),
                     reads=[b_rt1, b_rt2], writes=[b_rt1])
                pool(E.tensor_tensor(out=QrT[0:64, h, 0:NTOK], in0=rt1[0:64, 0:NTOK], in1=rq_bc[0:64, 0:NTOK], op=ALU.mult),
                     reads=[b_rt1, b_rq], writes=[b_Qr])
                bk, bb = gbank()
                mm(bk[:, 0:NTOK], w_kvb_b[:, h * 256:h * 256 + 128], kvlatT[:, 0:NTOK], True, True, [b_kvlat] + WA, [bb])
                P.op("dve", E.tensor_tensor(out=KnT[:, h, c0:c0 + NTOK], in0=bk[:, 0:NTOK], in1=rkv_bc[:, 0:NTOK], op=ALU.mult),
                     reads=[bb, b_rkv], writes=[b_K[gi]])

            def gla_gen(j):
                tl = tl0 + j
                tc = slice(j * 128, (j + 1) * 128)
                bk, bb = gbank()
                for k in range(8):
                    mm(bk[:, 0:256], uT[:, k, tc], w_in_b[:, k, 256:512], k == 0, k == 7, [b_uT] + WA, [bb])
                P.op("act", E.copy(out=ktok, in_=bk[:, 0:256]), reads=[bb], writes=[b_ktok])
                bk, bb = gbank()
                for k in range(8):
                    mm(bk, uT[:, k, tc], w_in_b[:, k, 512:1024], k == 0, k == 7, [b_uT] + WA, [bb])
                P.op("dve", E.tensor_copy(out=vtok, in_=bk), reads=[bb], writes=[b_vtok])
                bk, bb = gbank()
                mm(bk[:, 0:256], alowT[0:17, tc], w_a2_b[0:17, :], True, True, [b_alow] + WA, [bb])
                P.op("act", E.activation(out=la_e, in_=bk[:, 0:256], func=AF.Exp, scale=-1.0), reads=[bb], writes=[b_lae])
                P.op("act", E.activation(out=la, in_=la_e, func=AF.Ln, bias=1.0), reads=[b_lae], writes=[b_la])
                yield
                bkT, bbT = gbank()
                for pr in range(2):
                    mm(bkT[:, pr * 128:(pr + 1) * 128], la[:, pr * 128:(pr + 1) * 128], triN, True, True, [b_la] + C, [bbT])
                bkS, bbS = gbank()
                mm(bkS[:, 0:256], triUN, la, True, True, [b_la] + C, [bbS])
                P.op("act", E.activation(out=ebT, in_=bkT[:, 0:256].rearrange("p (a b) -> p a b", a=2), func=AF.Exp),
                     reads=[bbT], writes=[b_eb])
                P.op("act", E.activation(out=enbT, in_=bkT[:, 0:256].rearrange("p (a b) -> p a b", a=2), func=AF.Exp, scale=-1.0),
                     reads=[bbT], writes=[b_enb])
                P.op("act", E.activation(out=esuf, in_=bkS[:, 0:256], func=AF.Exp), reads=[bbS], writes=[b_esuf])
                P.op("dve", E.scalar_tensor_tensor(out=qeT, in0=qkT[:, 0:2, tc], scalar=0.125, in1=ebT, op0=ALU.mult, op1=ALU.mult),
                     reads=[b_qk, b_eb], writes=[b_qe])
                for i in range(2):
                    rr = slice(i * 64, i * 64 + 64)
                    P.op("dve", E.tensor_tensor(out=keTm[i][rr], in0=qkT[rr, 2:4, tc], in1=enbT[rr], op=ALU.mult), reads=[b_qk, b_enb], writes=[b_ke])
                    P.op("dve", E.tensor_tensor(out=kum[i][rr], in0=ktok[rr], in1=esuf[rr], op=ALU.mult), reads=[b_ktok, b_esuf], writes=[b_ku])
                yield
                bkA, bbA = gbank()
                for h in range(4):
                    pr, lo = h // 2, (h % 2) * 64
                    mm(bkA[:, h * 128:(h + 1) * 128], keTm[h % 2][:, pr, :], qeT[:, pr, :], True, True, [b_ke, b_qe], [bbA])
                P.op("dve", E.tensor_tensor(out=attm, in0=bkA.rearrange("p (h c) -> p h c", h=4),
                                                             in1=maskBD.unsqueeze(1).to_broadcast([128, 4, 128]), op=ALU.mult),
                     reads=[bbA] + C, writes=[b_attm])
                yield
                bkO, bbO = gbank()
                for h in range(4):
                    mm(bkO[:, h * 128:(h + 1) * 128], vtok[:, h * 128:(h + 1) * 128], attm[:, h, :], True, True, [b_vtok, b_attm], [bbO])
                bkZ, bbZ = gbank()
                for h in range(4):
                    pr, lo = h // 2, (h % 2) * 64
                    mm(bkZ[:, h * 128:h * 128 + 64], Sbm[h % 2][:, pr, :], qeT[:, pr, 0:64], True, True, [b_Sb, b_qe], [bbZ])
                for ch in range(2):
                    rows = slice(ch * 64, ch * 64 + 64)
                    bkU, bbU = gbank()
                    for h in range(4):
                        pr = h // 2
                        mm(bkU[:, h * 128:(h + 1) * 128], kum[ch][:, pr * 128:(pr + 1) * 128], vtok[:, h * 128:(h + 1) * 128], True, True,
                           [b_ku, b_vtok], [bbU])
                    col = ch * 64 + 63
                    for h in range(4):
                        pr, lo = h // 2, (h % 2) * 64
                        P.op("dve", E.scalar_tensor_tensor(
                            out=Sf[lo:lo + 64, pr, :], in0=Sf[lo:lo + 64, pr, :], scalar=ebT[lo:lo + 64, pr, col:col + 1],
                            in1=bkU[lo:lo + 64, h * 128:(h + 1) * 128], op0=ALU.mult, op1=ALU.add), reads=[bbU, b_eb, b_S], writes=[b_S])
                    if ch == 0:
                        for i in range(2):
                            pool(E.tensor_copy(out=SbBm[i][i * 64:i * 64 + 64], in_=Sf[i * 64:i * 64 + 64]), reads=[b_S], writes=[b_SbB])
                        for h in range(4):
                            pr, lo = h // 2, (h % 2) * 64
                            mm(bkZ[:, h * 128 + 64:h * 128 + 128], SbBm[h % 2][:, pr, :], qeT[:, pr, 64:128], True, True, [b_SbB, b_qe], [bbZ])
                    else:
                        for i in range(2):
                            pool(E.tensor_copy(out=Sbm[i][i * 64:i * 64 + 64], in_=Sf[i * 64:i * 64 + 64]), reads=[b_S], writes=[b_Sb])
                    yield
                P.op("act", E.copy(out=ost, in_=bkZ.rearrange("p (h c) -> p h c", h=4)), reads=[bbZ], writes=[b_ost])
                P.op("dve", E.tensor_tensor(out=oT, in0=bkO.rearrange("p (h c) -> p h c", h=4), in1=ost, op=ALU.add),
                     reads=[bbO, b_ost], writes=[b_oT])
                P.op("act", E.activation(out=osq, in_=oT, func=AF.Square), reads=[b_oT], writes=[b_osq])
                yield
                bk, bb = gbank()
                mm(bk, onesb, osq.rearrange("p h c -> p (h c)"), True, True, [b_osq] + C, [bb])
                P.op("act", E.activation(out=orr, in_=bk.rearrange("p (h c) -> p h c", h=4), func=AF.Sqrt, bias=epsb, scale=1.0 / 128),
                     reads=[bb] + C, writes=[b_orr])
                P.op("dve", E.reciprocal(out=orr, in_=orr), reads=[b_orr], writes=[b_orr])
                P.op("dve", E.tensor_tensor(out=oT, in0=oT, in1=orr, op=ALU.mult), reads=[b_oT, b_orr], writes=[b_oT])
                if j == ntl - 1:
                    pass
                P.op("dve", E.tensor_tensor(out=osq, in0=oT, in1=silr[:, :, tc], op=ALU.mult), reads=[b_oT, b_silr], writes=[b_osq])
                P.op("act", E.copy(out=mixT[:, 0:4, j * 128:(j + 1) * 128], in_=osq), reads=[b_osq], writes=[b_gla])
                yield

            for j in range(ntl):
                tl = tl0 + j
                tc = slice(j * 128, (j + 1) * 128)
                bk, bb = gbank()
                mm(bk[:, 0:8], sqkv[:, tc], onesb[:, 0:8], True, True, [b_sqkv] + C, [bb])
                P.op("act", E.activation(out=rkvt, in_=bk[:, 0:1], func=AF.Sqrt, bias=epsb, scale=1.0 / 128),
                     reads=[bb] + C, writes=[b_rkvt])
                P.op("dve", E.reciprocal(out=rkvt, in_=rkvt), reads=[b_rkvt], writes=[b_rkvt])
                bk, bb = gbank()
                for h in range(4):
                    mm(bk[:, h * 128:(h + 1) * 128], kvlatT[:, tc], w_kvb_b[:, h * 256 + 128:h * 256 + 256], True, True, [b_kvlat] + WA, [bb])
                P.op("act", E.activation(out=Vt[:, tl, :], in_=bk, func=AF.Copy, scale=rkvt[:, 0:1]),
                     reads=[bb, b_rkvt], writes=[b_K[gi]])
            gla_iter = itertools.chain(*[gla_gen(j) for j in range(ntl)])


            nfull = 1 if gi == 0 else 4 * gi - 3
            for h in range(4 if gi > 0 else 0):
                bO, bR = ps[:, 2, :], ps[:, 3, :]
                kblocks = [(kt, None) for kt in range(nfull)]
                if gi > 0:
                    kblocks += [(tl0 + d, d) for d in range(4)]
                nkb = len(kblocks)
                for bi, (kt, d) in enumerate(kblocks):
                    kg = 0 if kt == 0 else 1 + (kt - 1) // 4
                    q0 = 0 if d is None else 128 * d
                    si = st_rr[0] % 2
                    st_rr[0] += 1
                    bS, bbS_ = ps[:, si, :], psb[si]
                    mm(bS[:, q0:NTOK], KnT[:, h, kt * 128:(kt + 1) * 128], QnT[:, h, q0:NTOK], True, False, [b_K[kg], b_Qn], [bbS_])
                    mm(bS[:, q0:NTOK], KrT[:, kt * 128:(kt + 1) * 128], QrT[:, h, q0:NTOK], False, True, [b_K[kg], b_Qr], [bbS_])
                    if d is None:
                        pi = pt_rr[0] % 3
                        pt_rr[0] += 1
                        pt, bpt = PT[pi], b_PT[pi]
                        if kt == 0:
                            P.op("act", E.activation(out=pt[:, 0:NTOK], in_=bS[:, 0:NTOK], func=AF.Exp, bias=padbias),
                                 reads=[bbS_] + C, writes=[bpt])
                        else:
                            P.op("act", E.activation(out=pt[:, 0:NTOK], in_=bS[:, 0:NTOK], func=AF.Exp), reads=[bbS_], writes=[bpt])
                    else:
                        pt, bpt = PTd[d], b_PTd[d]
                        P.op("act", E.activation(out=pt[0:64, q0:512], in_=bS[0:64, q0:512], func=AF.Exp),
                             reads=[bbS_], writes=[bpt])
                        P.op("act", E.activation(out=pt[64:128, q0 + 64:512], in_=bS[64:128, q0 + 64:512], func=AF.Exp),
                             reads=[bbS_], writes=[bpt])
                    mm(bO[:, q0:NTOK], Vt[:, kt, h * 128:(h + 1) * 128], pt[:, q0:NTOK], bi == 0, bi == nkb - 1, [b_K[kg], bpt], [psb[2]])
                    mm(bR[:, q0:NTOK], onesb, pt[:, q0:NTOK], bi == 0, bi == nkb - 1, [bpt] + C, [psb[3]])
                    if bi % 2 == 1:
                        next(gla_iter, None)
                P.op("dve", E.reciprocal(out=rinv[:, 0:NTOK], in_=bR[:, 0:NTOK]), reads=[psb[3]], writes=[b_rinv])
                P.op("dve", E.tensor_tensor(out=mixT[:, 4 + h, 0:NTOK], in0=bO[:, 0:NTOK], in1=rinv[:, 0:NTOK], op=ALU.mult),
                     reads=[psb[2], b_rinv], writes=[b_mix])
            for _ in gla_iter:
                pass
            gidx = s * (NG + 1) + gi
            if gi > 0:
                P.dma("sp", "d_mix", E.dma_start(out=mixs[gidx], in_=mixT.rearrange("p a b -> p (a b)")), reads=[b_mix, b_gla], writes=[b_mixs[gidx]])

    P.barrier()
    ar.release()

    if phases < 2:
        return _finish(nc, st, P)
    ar.mark()
    w_out_b = ar.alloc([8, D], BF16)
    wr = ar.alloc([8, 72], F32)
    rb_bc = ar.alloc([72], F32)
    g2_bc = ar.alloc([D], F32)
    gon = ar.alloc([4], F32)
    tot = ar.alloc([64], F32); b_tot = Buf("tot")
    lim = ar.alloc([64], F32)
    toti = ar.alloc([64], I32)
    b_w2 = Buf("weightsA2")
    stage2 = ar.alloc([D], F32)
    with nc.allow_non_contiguous_dma(reason="tiny norm-gain vectors / router weights"):
        P.dma("sp", "d_w", E.dma_start(out=gon, in_=gla_on.rearrange("o (c p) -> p (o c)", p=128)), writes=[b_w2])
        P.dma("sp", "d_w", E.dma_start(out=wr[:, :, 0:8], in_=rgw[0].rearrange("(c p) n -> p c n", p=128)), writes=[b_w2])
        P.dma("sp", "d_w", E.dma_start(out=wr[:, :, 8:72], in_=rew[0].rearrange("(c p) n -> p c n", p=128)), writes=[b_w2])
        P.dma("sp", "d_w", E.dma_start(out=rb_bc[:, 0:8], in_=rgb.to_broadcast([128, 8])), writes=[b_w2])
        P.dma("sp", "d_w", E.dma_start(out=rb_bc[:, 8:72], in_=reb.to_broadcast([128, 64])), writes=[b_w2])
        P.dma("sp", "d_w", E.dma_start(out=g2_bc, in_=ffn_norm.to_broadcast([128, D])), writes=[b_w2])
    for c in range(8):
        P.dma("sp", "d_st", E.dma_start(out=stage2, in_=w_out[0, c * 128:(c + 1) * 128, :]), writes=[b_stage])
        if c < 4:
            P.op("dve", E.tensor_scalar(out=w_out_b[:, c, :], in0=stage2, scalar1=gon[:, c:c + 1], scalar2=None, op0=ALU.mult),
                 reads=[b_stage, b_w2], writes=[b_w2])
        else:
            P.op("dve", E.tensor_copy(out=w_out_b[:, c, :], in_=stage2), reads=[b_stage], writes=[b_w2])
    pool(E.iota(toti, pattern=[[CAP, 64]], base=0, channel_multiplier=0), writes=[b_tot])
    pool(E.tensor_copy(out=tot, in_=toti), reads=[b_tot], writes=[b_tot])
    pool(E.tensor_scalar(out=lim, in0=tot, scalar1=float(CAP - 1), scalar2=None, op0=ALU.add), reads=[b_tot], writes=[b_w2])
    W2 = [b_w2]

    mixg = ar.alloc([8, 512], BF16); b_mixg = Buf("mixg")
    u2b = [ar.alloc([D], BF16) for _ in range(2)]; b_u2b = [Buf("u2b0"), Buf("u2b1")]
    _sets = []
    for _i in range(2):
        _sets.append(dict(
            xt2=ar.alloc([D], F32), b_xt2=Buf("xt2_%d" % _i), hT=ar.alloc([D], F32), b_h=Buf("h_%d" % _i),
            junk2=ar.alloc([D], BF16), b_junk2=Buf("junk2_%d" % _i), u2f=ar.alloc([D], F32), b_u2f=Buf("u2f_%d" % _i),
            u2T=ar.alloc([8, 128], F32), b_u2T=Buf("u2T_%d" % _i), ss2=ar.alloc([1], F32), b_ss2=Buf("ss2_%d" % _i),
            lg=ar.alloc([72], F32), b_lg=Buf("lg_%d" % _i), sm=ar.alloc([64], F32), b_sm=Buf("sm_%d" % _i),
            t64=ar.alloc([64], F32), b_t64=Buf("t64_%d" % _i), A1=ar.alloc([64], F32), b_A1=Buf("A1_%d" % _i),
            A2=ar.alloc([64], F32), b_A2=Buf("A2_%d" % _i), Ab=ar.alloc([64], BF16), b_Ab=Buf("Ab_%d" % _i),
            posc=ar.alloc([64], F32), b_posc=Buf("posc_%d" % _i)))
    b_XS = Buf("XS")
    b_out = [Buf("out%d" % i) for i in range(NT)]
    zt = ar.alloc([8192], BF16); b_zt = Buf("zt")
    pool(E.memset(zt, 0.0), writes=[b_zt])
    RPP = NSLOT // 128
    XSz = XS.rearrange("(p r) d -> p (r d)", p=128)
    for c in range(RPP // 8):
        P.dma("sp", "d_z", E.dma_start(out=XSz[:, c * 8192:(c + 1) * 8192], in_=zt), reads=[b_zt], writes=[b_XS])

    for s in range(NSEQ):
        for g in range(1, NG + 1):
            gidx = s * (NG + 1) + g
            P.dma("sp", "d_mg", E.dma_start(out=mixg.rearrange("p a b -> p (a b)"), in_=mixs[gidx]), reads=[b_mixs[gidx]], writes=[b_mixg])
            for j in range(4):
                ti = s * 4 * NG + (g - 1) * 4 + j
                _d = _sets[ti % 2]
                xt2, b_xt2, hT, b_h, junk2, b_junk2, u2f, b_u2f = _d["xt2"], _d["b_xt2"], _d["hT"], _d["b_h"], _d["junk2"], _d["b_junk2"], _d["u2f"], _d["b_u2f"]
                u2T, b_u2T, ss2, b_ss2, lg, b_lg, sm, b_sm = _d["u2T"], _d["b_u2T"], _d["ss2"], _d["b_ss2"], _d["lg"], _d["b_lg"], _d["sm"], _d["b_sm"]
                t64, b_t64, A1, b_A1, A2, b_A2, Ab, b_Ab, posc, b_posc = _d["t64"], _d["b_t64"], _d["A1"], _d["b_A1"], _d["A2"], _d["b_A2"], _d["Ab"], _d["b_Ab"], _d["posc"], _d["b_posc"]
                r0 = (g - 1) * 512 + j * 128
                tc = slice(j * 128, (j + 1) * 128)
                P.dma("sp", "d_x2_%d" % (ti % 2), E.dma_start(out=xt2, in_=x[s, r0:r0 + 128, :]), writes=[b_xt2])
                for half in range(2):
                    bk, bb = gbank()
                    for k in range(8):
                        mm(bk, mixg[:, k, tc], w_out_b[:, k, half * 512:(half + 1) * 512], k == 0, k == 7, [b_mixg] + W2, [bb])
                    P.op("dve", E.tensor_tensor(out=hT[:, half * 512:(half + 1) * 512], in0=bk,
                                                                           in1=xt2[:, half * 512:(half + 1) * 512], op=ALU.add),
                         reads=[bb, b_xt2], writes=[b_h])
                P.dma("sp", "d_h%d" % (ti % 2), E.dma_start(out=out[ti * 128:(ti + 1) * 128, :], in_=hT), reads=[b_h], writes=[b_out[ti]])
                P.op("act", E.activation(out=junk2, in_=hT, func=AF.Square, accum_out=ss2), reads=[b_h], writes=[b_junk2, b_ss2])
                rstd_from_ss(ss2, 1.0 / D, b_ss2)
                P.op("dve", E.scalar_tensor_tensor(out=u2f, in0=hT, scalar=ss2[:, 0:1], in1=g2_bc, op0=ALU.mult, op1=ALU.mult),
                     reads=[b_h, b_ss2] + W2, writes=[b_u2f])
                ub, bub = u2b[ti % 2], b_u2b[ti % 2]
                P.op("act", E.copy(out=ub, in_=u2f), reads=[b_u2f], writes=[bub])
                for half in range(2):
                    bk, bb = gbank()
                    for k in range(4):
                        kk = half * 4 + k
                        P.op("pe", E.transpose(out=bk[:, k * 128:(k + 1) * 128], in_=u2f[:, kk * 128:(kk + 1) * 128], identity=identf),
                             reads=[b_u2f] + C, writes=[bb])
                    P.op("act", E.copy(out=u2T[:, half * 4:half * 4 + 4, :], in_=bk.rearrange("p (k t) -> p k t", k=4)),
                         reads=[bb], writes=[b_u2T])
                bk, bb = gbank()
                for k in range(8):
                    mm(bk[:, 0:72], u2T[:, k, :], wr[:, k, :], k == 0, k == 7, [b_u2T] + W2, [bb])
                P.op("dve", E.tensor_tensor(out=lg, in0=bk[:, 0:72], in1=rb_bc, op=ALU.add), reads=[bb] + W2, writes=[b_lg])
                gl = lg[:, 0:8]
                el3 = lg[:, 8:72].rearrange("p (g e) -> p g e", g=8)
                m8g, m8e = sm[:, 0:8], sm[:, 8:16]
                ohg, sel, oh1, oh2 = sm[:, 16:24], sm[:, 24:32], sm[:, 32:40], sm[:, 40:48]
                ngm, gsum, dd, tt = sm[:, 48:49], sm[:, 49:50], sm[:, 50:51], sm[:, 51:52]
                s1f, s2f = sm[:, 52:53], sm[:, 53:54]
                eg = sm[:, 56:64]
                R_, W_ = [b_lg, b_sm], [b_sm]

                def dv(fn, reads=R_, writes=W_):
                    P.op("dve", fn, reads=reads, writes=writes)
                dv(E.max(out=m8g, in_=gl))
                dv(E.tensor_scalar(out=ohg, in0=gl, scalar1=m8g[:, 0:1], scalar2=None, op0=ALU.is_equal))
                dv(E.tensor_scalar(out=ngm, in0=m8g[:, 0:1], scalar1=-1.0, scalar2=None, op0=ALU.mult))
                P.op("act", E.activation(out=eg, in_=gl, func=AF.Exp, bias=ngm, accum_out=gsum), reads=R_, writes=W_)
                t3 = t64.rearrange("p (g e) -> p g e", g=8)
                dv(E.tensor_tensor(out=t3, in0=el3, in1=ohg.unsqueeze(2).to_broadcast([128, 8, 8]), op=ALU.mult), writes=[b_t64])
                dv(E.tensor_reduce(out=sel, in_=t64.rearrange("p (g e) -> p e g", g=8), axis=AX.X, op=ALU.add), reads=[b_t64, b_sm])
                dv(E.max(out=m8e, in_=sel))
                dv(E.tensor_scalar(out=oh1, in0=sel, scalar1=m8e[:, 0:1], scalar2=None, op0=ALU.is_equal))
                dv(E.tensor_scalar(out=oh2, in0=sel, scalar1=m8e[:, 1:2], scalar2=None, op0=ALU.is_equal))
                dv(E.tensor_tensor(out=dd, in0=m8e[:, 1:2], in1=m8e[:, 0:1], op=ALU.subtract))
                P.op("act", E.activation(out=dd, in_=dd, func=AF.Exp), reads=R_, writes=W_)
                dv(E.tensor_scalar(out=tt, in0=dd, scalar1=1.0, scalar2=gsum, op0=ALU.add, op1=ALU.mult))
                dv(E.reciprocal(out=gates[:, ti, 0:1], in_=tt), writes=[b_sm, b_gates[ti]])
                dv(E.tensor_tensor(out=gates[:, ti, 1:2], in0=gates[:, ti, 0:1], in1=dd, op=ALU.mult), writes=[b_sm, b_gates[ti]])
                A13 = A1.rearrange("p (g e) -> p g e", g=8)
                A23 = A2.rearrange("p (g e) -> p g e", g=8)
                dv(E.tensor_tensor(out=A13, in0=ohg.unsqueeze(2).to_broadcast([128, 8, 8]), in1=oh1.unsqueeze(1).to_broadcast([128, 8, 8]), op=ALU.mult),
                   writes=[b_A1])
                dv(E.tensor_tensor(out=A23, in0=ohg.unsqueeze(2).to_broadcast([128, 8, 8]), in1=oh2.unsqueeze(1).to_broadcast([128, 8, 8]), op=ALU.mult),
                   writes=[b_A2])
                dv(E.tensor_tensor(out=Ab, in0=A1, in1=A2, op=ALU.add), reads=[b_A1, b_A2], writes=[b_Ab])
                bkc, bbc = gbank()
                mm(bkc[:, 0:64], Ub, Ab, True, True, [b_Ab] + C, [bbc])
                mm(bkc[:, 64:128], onesb, Ab, True, True, [b_Ab] + C, [bbc])
                dv(E.tensor_tensor(out=posc, in0=bkc[:, 0:64], in1=tot, op=ALU.add), reads=[bbc, b_tot], writes=[b_posc])
                dv(E.tensor_tensor(out=posc, in0=posc, in1=lim, op=ALU.min), reads=[b_posc] + W2, writes=[b_posc])
                dv(E.tensor_tensor(out=tot, in0=tot, in1=bkc[:, 64:128], op=ALU.add), reads=[bbc, b_tot, b_posc], writes=[b_tot])
                dv(E.tensor_tensor(out=t64, in0=A1, in1=posc, op=ALU.mult), reads=[b_A1, b_posc], writes=[b_t64])
                dv(E.tensor_reduce(out=s1f, in_=t64, axis=AX.X, op=ALU.add), reads=[b_t64, b_sm])
                dv(E.tensor_tensor(out=t64, in0=A2, in1=posc, op=ALU.mult), reads=[b_A2, b_posc, b_sm], writes=[b_t64])
                dv(E.tensor_reduce(out=s2f, in_=t64, axis=AX.X, op=ALU.add), reads=[b_t64, b_sm])
                dv(E.tensor_copy(out=slots[:, ti, 0:2], in_=sm[:, 52:54]), writes=[b_sm, b_gates[ti]])
                for kk in range(2):
                    P.dma("pool", "d_sc%d" % (ti % 2), E.indirect_dma_start(
                        out=XS, out_offset=bass.IndirectOffsetOnAxis(ap=slots[:, ti, kk:kk + 1], axis=0), in_=ub, in_offset=None),
                        reads=[bub, b_gates[ti]], writes=[b_XS])
    P.barrier()
    ar.release()

    if dbg:
        dg = nc.dram_tensor('dbg_gates', [128, NT * 2], F32, kind='ExternalOutput').ap()
        dsl = nc.dram_tensor('dbg_slots', [128, NT * 2], I32, kind='ExternalOutput').ap()
        P.dma('sp', 'd_dbg', E.dma_start(out=dg, in_=gates.rearrange('p a b -> p (a b)')), reads=b_gates)
        P.dma('sp', 'd_dbg', E.dma_start(out=dsl, in_=slots.rearrange('p a b -> p (a b)')), reads=b_gates)
        P.barrier()
    if phases < 3:
        return _finish(nc, st, P)
    ar.mark()
    NBUF = 2
    wst = [[ar.alloc([8, 512], F32), ar.alloc([8, 512], F32), ar.alloc([4, D], F32)] for _ in range(NBUF)]
    b_wst = [[Buf("wst%d_%d" % (i, j)) for j in range(3)] for i in range(NBUF)]
    wbf = [[ar.alloc([8, 512], BF16), ar.alloc([8, 512], BF16), ar.alloc([4, D], BF16)] for _ in range(NBUF)]
    b_wbf = [[Buf("wbf%d_%d" % (i, j)) for j in range(3)] for i in range(NBUF)]
    xsl = [ar.alloc([D], BF16) for _ in range(2)]; b_xsl = [Buf("xsl0"), Buf("xsl1")]
    XT = [ar.alloc([8, CAP], BF16) for _ in range(2)]; b_XT = [Buf("XT0"), Buf("XT1")]
    sg = [ar.alloc([CAP], F32) for _ in range(2)]; b_sg = [Buf("sg0"), Buf("sg1")]
    hTb = [ar.alloc([4, CAP], BF16) for _ in range(2)]; b_hTb = [Buf("hTb0"), Buf("hTb1")]
    ybuf = [ar.alloc([D], F32) for _ in range(2)]; b_ybuf = [Buf("ybuf0"), Buf("ybuf1")]
    b_YS = Buf("YS")
    yrr = [0]
    xrr = [0]

    def load_expert(ex):
        i = ex % NBUF
        P.dma("sp", "d_wg%d" % i, E.dma_start(out=wst[i][0], in_=ewg[0, ex].rearrange("(c p) n -> p c n", p=128)), writes=[b_wst[i][0]])
        P.dma("sp", "d_wu%d" % i, E.dma_start(out=wst[i][1], in_=ewu[0, ex].rearrange("(c p) n -> p c n", p=128)), writes=[b_wst[i][1]])
        P.dma("sp", "d_wd%d" % i, E.dma_start(out=wst[i][2], in_=ewd[0, ex].rearrange("(c p) n -> p c n", p=128)), writes=[b_wst[i][2]])

    def cast_expert(ex):
        i = ex % NBUF
        P.op("dve", E.tensor_copy(out=wbf[i][0], in_=wst[i][0]), reads=[b_wst[i][0]], writes=[b_wbf[i][0]])
        P.op("pool", E.tensor_copy(out=wbf[i][1], in_=wst[i][1]), reads=[b_wst[i][1]], writes=[b_wbf[i][1]])
        P.op("act", E.copy(out=wbf[i][2], in_=wst[i][2]), reads=[b_wst[i][2]], writes=[b_wbf[i][2]])

    load_expert(0)
    for ex in range(NE):
        i = ex % NBUF
        cast_expert(ex)
        if ex + 1 < NE:
            load_expert(ex + 1)
        xi = ex % 2
        for sb in range(NB):
            xs_i = xrr[0] % 2
            xrr[0] += 1
            r0 = ex * CAP + sb * 128
            P.dma("act", "d_xs%d" % xs_i, E.dma_start(out=xsl[xs_i], in_=XS[r0:r0 + 128, :]), reads=[b_XS], writes=[b_xsl[xs_i]])
            bk, bb = gbank()
            bkb = bk.bitcast(BF16)
            for k in range(8):
                P.op("pe", E.transpose(out=bkb[:, k * 128:(k + 1) * 128], in_=xsl[xs_i][:, k * 128:(k + 1) * 128], identity=identb),
                     reads=[b_xsl[xs_i]] + C, writes=[bb])
            P.op("dve", E.tensor_copy(out=XT[xi][:, :, sb * 128:(sb + 1) * 128], in_=bkb.rearrange("p (k t) -> p k t", k=8)),
                 reads=[bb], writes=[b_XT[xi]])
        for jh in range(4):
            hi = jh % 2
            bkG, bbG = gbank()
            for k in range(8):
                mm(bkG[:, 0:CAP], wbf[i][0][:, k, jh * 128:(jh + 1) * 128], XT[xi][:, k, :], k == 0, k == 7, [b_wbf[i][0], b_XT[xi]], [bbG])
            bkU, bbU = gbank()
            for k in range(8):
                mm(bkU[:, 0:CAP], wbf[i][1][:, k, jh * 128:(jh + 1) * 128], XT[xi][:, k, :], k == 0, k == 7, [b_wbf[i][1], b_XT[xi]], [bbU])
            P.op("act", E.activation(out=sg[hi], in_=bkG[:, 0:CAP], func=AF.Silu), reads=[bbG], writes=[b_sg[hi]])
            P.op("dve", E.tensor_tensor(out=hTb[xi][:, jh, :], in0=bkU[:, 0:CAP], in1=sg[hi], op=ALU.mult),
                 reads=[bbU, b_sg[hi]], writes=[b_hTb[xi]])
        for sb in range(NB):
            yi = yrr[0] % 2
            yrr[0] += 1
            for half in range(2):
                bk, bb = gbank()
                for jh in range(4):
                    mm(bk, hTb[xi][:, jh, sb * 128:(sb + 1) * 128], wbf[i][2][:, jh, half * 512:(half + 1) * 512], jh == 0, jh == 3,
                       [b_hTb[xi], b_wbf[i][2]], [bb])
                if half == 0:
                    P.op("act", E.copy(out=ybuf[yi][:, 0:512], in_=bk), reads=[bb], writes=[b_ybuf[yi]])
                else:
                    P.op("dve", E.tensor_copy(out=ybuf[yi][:, 512:1024], in_=bk), reads=[bb], writes=[b_ybuf[yi]])
            r0 = ex * CAP + sb * 128
            P.dma("sp", "d_ys%d" % yi, E.dma_start(out=YS[r0:r0 + 128, :], in_=ybuf[yi]), reads=[b_ybuf[yi]], writes=[b_YS])
    P.barrier()
    ar.release()

    if phases < 4:
        return _finish(nc, st, P)
    ar.mark()
    gf_bc = ar.alloc([D], F32); b_gf = Buf("gf")
    with nc.allow_non_contiguous_dma(reason="broadcast norm gain"):
        P.dma("sp", "d_w", E.dma_start(out=gf_bc, in_=final_norm.to_broadcast([128, D])), writes=[b_gf])
    hb = [ar.alloc([D], F32) for _ in range(2)]; b_hb = [Buf("hb0"), Buf("hb1")]
    y1 = [ar.alloc([D], F32) for _ in range(2)]; b_y1 = [Buf("y1_0"), Buf("y1_1")]
    y2 = [ar.alloc([D], F32) for _ in range(2)]; b_y2 = [Buf("y2_0"), Buf("y2_1")]
    ob = [ar.alloc([D], F32) for _ in range(2)]; b_ob = [Buf("ob0"), Buf("ob1")]
    junk3 = ar.alloc([D], F32); b_junk3 = Buf("junk3")
    ss3 = [ar.alloc([1], F32) for _ in range(2)]; b_ss3 = [Buf("ss3_0"), Buf("ss3_1")]
    for ti in range(NT):
        i = ti % 2
        P.dma("sp", "d_hb%d" % i, E.dma_start(out=hb[i], in_=out[ti * 128:(ti + 1) * 128, :]), reads=[b_out[ti]], writes=[b_hb[i]])
        P.dma("pool", "d_g1%d" % i, E.indirect_dma_start(
            out=y1[i], out_offset=None, in_=YS, in_offset=bass.IndirectOffsetOnAxis(ap=slots[:, ti, 0:1], axis=0)), reads=[b_YS, b_gates[ti]], writes=[b_y1[i]])
        P.dma("pool", "d_g2%d" % i, E.indirect_dma_start(
            out=y2[i], out_offset=None, in_=YS, in_offset=bass.IndirectOffsetOnAxis(ap=slots[:, ti, 1:2], axis=0)), reads=[b_YS, b_gates[ti]], writes=[b_y2[i]])
        P.op("dve", E.scalar_tensor_tensor(out=hb[i], in0=y1[i], scalar=gates[:, ti, 0:1], in1=hb[i], op0=ALU.mult, op1=ALU.add),
             reads=[b_y1[i], b_hb[i], b_gates[ti]], writes=[b_hb[i]])
        P.op("dve", E.scalar_tensor_tensor(out=hb[i], in0=y2[i], scalar=gates[:, ti, 1:2], in1=hb[i], op0=ALU.mult, op1=ALU.add),
             reads=[b_y2[i], b_hb[i], b_gates[ti]], writes=[b_hb[i]])
        P.op("act", E.activation(out=junk3, in_=hb[i], func=AF.Square, accum_out=ss3[i]), reads=[b_hb[i]], writes=[b_junk3, b_ss3[i]])
        rstd_from_ss(ss3[i], 1.0 / D, b_ss3[i])
        P.op("dve", E.scalar_tensor_tensor(out=ob[i], in0=hb[i], scalar=ss3[i][:, 0:1], in1=gf_bc, op0=ALU.mult, op1=ALU.mult),
             reads=[b_hb[i], b_ss3[i], b_gf], writes=[b_ob[i]])
        P.dma("sp", "d_o%d" % i, E.dma_start(out=out[ti * 128:(ti + 1) * 128, :], in_=ob[i]), reads=[b_ob[i], b_hb[i]], writes=[b_out[ti]])
    P.barrier()
    ar.release()
    return _finish(nc, st, P)


def _finish(nc, st, P):
    P.barrier()
    P.emit(st)
    st.close()
    return nc, st, P


def rope_table(NG):
    T = 1 + 4 * NG
    LP = 128 * T
    pos = (np.arange(LP, dtype=np.float32) - np.float32(112.0)).astype(np.float32)
    inv = (np.float32(10000.0) ** (-np.arange(0, 64, 2, dtype=np.float32) / np.float32(64))).astype(np.float32)
    ang = (pos[None, :] * inv[:, None]).astype(np.float32)
    cs = np.zeros((64, 2, LP), np.float32)
    cs[0:32, 0] = np.cos(ang); cs[32:64, 0] = np.cos(ang)
    cs[0:32, 1] = np.sin(ang); cs[32:64, 1] = np.sin(ang)
    return cs


_CACHE = {}


def run(inputs, n_cores, NSEQ, NG, CAP, dbg=False, phases=4):
    key = (NSEQ, NG, CAP, dbg, phases)
    if key not in _CACHE:
        _CACHE[key] = build_program(NSEQ, NG, CAP, dbg, phases)
    nc = _CACHE[key]
    x = np.ascontiguousarray(inputs["x"], dtype=np.float32)
    cs = rope_table(NG)
    in_maps = []
    for c in range(n_cores):
        m = {"x": x[c * NSEQ:(c + 1) * NSEQ], "rope_cs": cs}
        for k, v in inputs.items():
            if k == "x":
                continue
            a = np.ascontiguousarray(v, dtype=np.float32)
            if k == "final_norm":
                a = a.reshape(1, -1)
            m[k] = a
        in_maps.append(m)
    res = run_bass_kernel_spmd(nc, in_maps, core_ids=list(range(n_cores)))
    return res


def kernel(**inputs):
    x = inputs["x"]
    B, S, Dm = x.shape
    res = run(inputs, 8, 2, 8, 384)
    outs = [r["out"].reshape(2, S, Dm) for r in res.results]
    return np.concatenate(outs, axis=0).astype(np.float32)
```

```python
import itertools
import numpy as np
from contextlib import ExitStack
import concourse.bass as bass
import concourse.mybir as mybir
from concourse.bass_utils import run_bass_kernel_spmd

F32 = mybir.dt.float32
BF16 = mybir.dt.bfloat16
I32 = mybir.dt.int32
AF = mybir.ActivationFunctionType
ALU = mybir.AluOpType
AX = mybir.AxisListType
ENGS = ("pe", "act", "dve", "pool", "sp")
EPS = 1e-6


class Buf:
    __slots__ = ("name", "lw", "rd")

    def __init__(self, name):
        self.name = name
        self.lw = None
        self.rd = {}


class Call:
    __slots__ = ("name", "args", "kwargs")

    def __init__(self, name, args, kwargs):
        self.name, self.args, self.kwargs = name, args, kwargs


class _Rec:
    def __getattr__(self, name):
        def f(*args, **kwargs):
            return Call(name, args, kwargs)
        return f


E = _Rec()


class Prog:
    def __init__(self, nc):
        self.nc = nc
        self.ops = {e: [] for e in ENGS}
        self.cnt = {e: 0 for e in ENGS}
        self.seen = {e: {} for e in ENGS}
        self.sems = {}
        self.dcnt = {}
        import os
        self.limit = int(os.environ.get("KLIMIT", "100000000"))

    def _deps(self, eng, reads, writes):
        ev = {}

        def need(e):
            if e is None:
                return
            k, v = e
            if ev.get(k, 0) < v:
                ev[k] = v
        for b in reads:
            need(b.lw)
        for b in writes:
            need(b.lw)
            for k, v in b.rd.items():
                need((k, v))
        waits = []
        for k, v in ev.items():
            if k == eng and eng in ("pe", "sp"):
                continue
            if self.seen[eng].get(k, 0) >= v:
                continue
            self.seen[eng][k] = v
            waits.append((k, v))
        return waits

    def _commit(self, event, reads, writes):
        k, v = event
        for b in reads:
            if b.rd.get(k, 0) < v:
                b.rd[k] = v
        for b in writes:
            b.lw = event
            b.rd = {}

    def op(self, eng, fn, reads=(), writes=()):
        self.nrec = getattr(self, "nrec", 0) + 1
        if self.nrec > self.limit:
            return
        waits = self._deps(eng, reads, writes)
        self.cnt[eng] += 1
        self.ops[eng].append((waits, fn, (eng, 1)))
        self._commit((eng, self.cnt[eng]), reads, writes)

    def dma(self, eng, dsem, fn, reads=(), writes=()):
        self.nrec = getattr(self, "nrec", 0) + 1
        if self.nrec > self.limit:
            return
        waits = self._deps(eng, reads, writes)
        self.dcnt[dsem] = self.dcnt.get(dsem, 0) + 16
        self.ops[eng].append((waits, fn, (dsem, 16)))
        self._commit((dsem, self.dcnt[dsem]), reads, writes)

    def barrier(self):
        for e in ENGS:
            waits = []
            for k in ENGS:
                if k != e and self.cnt[k] > self.seen[e].get(k, 0):
                    self.seen[e][k] = self.cnt[k]
                    waits.append((k, self.cnt[k]))
            for k, v in self.dcnt.items():
                if v > self.seen[e].get(k, 0):
                    self.seen[e][k] = v
                    waits.append((k, v))
            self.ops[e].append((waits, None, None))

    def emit(self, stack):
        nc = self.nc
        keys = set(ENGS) | set(self.dcnt.keys())
        for k in sorted(keys):
            self.sems[k] = stack.enter_context(nc.semaphore("s_" + k))
        stack.enter_context(nc.allow_non_contiguous_dma(reason="tiny gain vectors / router weights / broadcasts"))
        block = stack.enter_context(nc.Block())
        sems = self.sems

        def runner(e):
            lst = self.ops[e]

            def body(engobj):
                for waits, fn, inc in lst:
                    for k, v in waits:
                        engobj.wait_ge(sems[k], v)
                    if fn is not None:
                        getattr(engobj, fn.name)(*fn.args, **fn.kwargs).then_inc(sems[inc[0]], inc[1])
            return body
        block.tensor(runner("pe"))
        block.scalar(runner("act"))
        block.vector(runner("dve"))
        block.gpsimd(runner("pool"))
        block.sync(runner("sp"))


class Arena:
    def __init__(self, nc, stack, nbytes):
        self.t = stack.enter_context(nc.sbuf_tensor("arena", [128, nbytes // 4], F32))
        self.nbytes = nbytes
        self.off = 0
        self.marks = []
        self.peak = 0

    def alloc(self, shape, dtype, parts=128):
        esz = 2 if dtype == BF16 else 4
        n = int(np.prod(shape))
        nb = (n * esz + 63) // 64 * 64
        assert self.off + nb <= self.nbytes, f"arena overflow {self.off}+{nb}>{self.nbytes}"
        a, b = self.off // 4, (self.off + nb) // 4
        self.off += nb
        self.peak = max(self.peak, self.off)
        ap = self.t[0:parts, a:b]
        if dtype != F32:
            ap = ap.bitcast(dtype)
        ap = ap[:, 0:n]
        if len(shape) == 2:
            ap = ap.rearrange("p (a b) -> p a b", a=shape[0])
        elif len(shape) == 3:
            ap = ap.rearrange("p (a b c) -> p a b c", a=shape[0], b=shape[1])
        return ap

    def mark(self):
        self.marks.append(self.off)

    def release(self):
        self.off = self.marks.pop()


D = 1024
DIN = 2000
NE = 64
SCALE = (128 + 64) ** -0.5


class _Stop(Exception):
    pass


def build_program(NSEQ=2, NG=8, CAP=384, dbg=False, phases=4):
    nc, st, P = _build_body(NSEQ, NG, CAP, dbg, phases)
    return nc


def _build_body(NSEQ, NG, CAP, dbg, phases):
    nc = bass.Bass("TRN2", target_bir_lowering=False)
    T = 1 + 4 * NG
    LP = 128 * T
    NF = 512 * NG
    NT = NSEQ * 4 * NG
    NSLOT = NE * CAP
    NB = CAP // 128

    def din(name, shape, dt=F32):
        return nc.dram_tensor(name, list(shape), dt, kind="ExternalInput").ap()
    x = din("x", [NSEQ, NF, D])
    meta = din("meta_tokens", [16, D])
    mix_norm = din("mix_norm", [1, D])
    w_in = din("w_in", [1, D, DIN])
    w_a2 = din("gla_w_a2", [1, 16, 256])
    b_a = din("gla_b_a", [1, 256])
    gla_on = din("gla_out_norm", [1, 512])
    q_norm = din("mla_q_norm", [1, 256])
    w_qb = din("mla_w_qb", [1, 256, 768])
    kv_norm = din("mla_kv_norm", [1, 128])
    w_kvb = din("mla_w_kvb", [1, 128, 1024])
    w_out = din("w_out", [1, D, D])
    ffn_norm = din("ffn_norm", [1, D])
    rgw = din("router_group_w", [1, D, 8])
    rgb = din("router_group_b", [1, 8])
    rew = din("router_expert_w", [1, D, 64])
    reb = din("router_expert_b", [1, 64])
    ewg = din("expert_w_gate", [1, NE, D, 512])
    ewu = din("expert_w_up", [1, NE, D, 512])
    ewd = din("expert_w_down", [1, NE, 512, D])
    final_norm = din("final_norm", [1, D])
    rope = din("rope_cs", [64, 2, LP])
    out = nc.dram_tensor("out", [NSEQ * NF, D], F32, kind="ExternalOutput").ap()
    skind = "ExternalOutput" if dbg else "Internal"
    mixs = nc.dram_tensor("mixs", [NSEQ * (NG + 1), 128, 8 * 512], BF16, kind=skind).ap()
    XS = nc.dram_tensor("xs_scr", [NSLOT, D], BF16, kind="Internal").ap()
    YS = nc.dram_tensor("ys_scr", [NSLOT, D], F32, kind="Internal").ap()

    st = ExitStack()
    P = Prog(nc)
    import os
    ar = Arena(nc, st, int(os.environ.get('KARENA', '205')) * 1024)
    ps = st.enter_context(nc.psum_tensor("ps", [128, 8, 512], F32))
    psb = [Buf("psb%d" % i) for i in range(8)]
    gen_rr = [0]

    def gbank():
        i = 4 + gen_rr[0] % 4
        gen_rr[0] += 1
        return ps[:, i, :], psb[i]

    def mm(out_ap, lhsT, rhs, start, stop, reads, writes):
        P.op("pe", E.matmul(out_ap, lhsT=lhsT, rhs=rhs, start=start, stop=stop), reads=reads, writes=writes)

    identf = ar.alloc([128], F32)
    identb = ar.alloc([128], BF16)
    onesb = ar.alloc([128], BF16)
    onesf = ar.alloc([128], F32)
    Ub = ar.alloc([128], BF16)
    triN = ar.alloc([128], F32)
    triUN = ar.alloc([128], F32)
    maskBD = ar.alloc([128], F32)
    padbias = ar.alloc([1], F32)
    epsb = ar.alloc([1], F32)
    tmpc = ar.alloc([128], F32)
    gates = ar.alloc([NT, 2], F32)
    slots = ar.alloc([NT, 2], I32)
    b_const = Buf("const")
    b_gates = [Buf("gates%d" % i) for i in range(NT)]

    def pool(fn, reads=(), writes=()):
        P.op("pool", fn, reads=reads, writes=writes)

    W = [b_const]
    pool(E.memset(identf, 0.0), writes=W)
    pool(E.affine_select(out=identf, in_=identf, pattern=[[-1, 128]], compare_op=ALU.not_equal,
                                   fill=1.0, base=0, channel_multiplier=1), writes=W)
    pool(E.tensor_copy(out=identb, in_=identf), writes=W)
    pool(E.memset(onesf, 1.0), writes=W)
    pool(E.memset(onesb, 1.0), writes=W)
    pool(E.memset(epsb, EPS), writes=W)
    pool(E.affine_select(out=tmpc, in_=onesf, pattern=[[1, 128]], compare_op=ALU.is_ge,
                                   fill=0.0, base=-1, channel_multiplier=-1), writes=W)
    pool(E.tensor_copy(out=Ub, in_=tmpc), writes=W)
    pool(E.affine_select(out=maskBD, in_=onesf, pattern=[[1, 128]], compare_op=ALU.is_ge,
                                   fill=0.0, base=0, channel_multiplier=-1), writes=W)
    pool(E.memset(maskBD[0:64, 64:128], 0.0), writes=W)
    pool(E.tensor_scalar(out=triN, in0=maskBD, scalar1=-1.0 / 16.0, scalar2=None, op0=ALU.mult), writes=W)
    pool(E.affine_select(out=triUN, in_=onesf, pattern=[[-1, 128]], compare_op=ALU.is_ge,
                                   fill=0.0, base=-1, channel_multiplier=1), writes=W)
    pool(E.memset(triUN[64:128, 0:64], 0.0), writes=W)
    pool(E.tensor_scalar(out=triUN, in0=triUN, scalar1=-1.0 / 16.0, scalar2=None, op0=ALU.mult), writes=W)
    pool(E.memset(padbias, -30000.0), writes=W)
    pool(E.affine_select(out=padbias, in_=padbias, pattern=[[0, 1]], compare_op=ALU.is_ge,
                                   fill=0.0, base=111, channel_multiplier=-1), writes=W)
    C = [b_const]

    def rstd_from_ss(ss_ap, n_inv, bss):
        P.op("act", E.activation(out=ss_ap, in_=ss_ap, func=AF.Sqrt, bias=epsb[0:ss_ap.shape[0], :], scale=n_inv),
             reads=[bss] + C, writes=[bss])
        P.op("dve", E.reciprocal(out=ss_ap, in_=ss_ap), reads=[bss], writes=[bss])

    ar.mark()
    NCI = DIN + 64
    w_in_b = ar.alloc([8, NCI], BF16)
    w_qb_b = ar.alloc([2, 1024], BF16)
    w_kvb_b = ar.alloc([1024], BF16)
    w_a2_b = ar.alloc([256], BF16)
    gin = ar.alloc([8], F32)
    gqn = ar.alloc([2], F32)
    gkv = ar.alloc([1], F32)
    b_w = Buf("weightsA")
    ar.mark()
    stage = ar.alloc([DIN], F32)
    b_stage = Buf("stage")
    with nc.allow_non_contiguous_dma(reason="tiny norm-gain vectors"):
        P.dma("sp", "d_w", E.dma_start(out=gin, in_=mix_norm.rearrange("o (c p) -> p (o c)", p=128)), writes=[b_w])
        P.dma("sp", "d_w", E.dma_start(out=gqn, in_=q_norm.rearrange("o (c p) -> p (o c)", p=128)), writes=[b_w])
        P.dma("sp", "d_w", E.dma_start(out=gkv, in_=kv_norm.rearrange("o (c p) -> p (o c)", p=128)), writes=[b_w])
    for c in range(8):
        P.dma("sp", "d_st", E.dma_start(out=stage, in_=w_in[0, c * 128:(c + 1) * 128, :]), writes=[b_stage])
        P.op("dve", E.tensor_scalar(out=w_in_b[:, c, 0:DIN], in0=stage, scalar1=gin[:, c:c + 1], scalar2=None, op0=ALU.mult),
             reads=[b_stage, b_w], writes=[b_w])
        P.op("dve", E.tensor_scalar(out=w_in_b[:, c, DIN:DIN + 32], in0=stage[:, 1968:2000], scalar1=gin[:, c:c + 1], scalar2=-1.0,
                                                   op0=ALU.mult, op1=ALU.mult), reads=[b_stage, b_w], writes=[b_w])
        P.op("dve", E.tensor_scalar(out=w_in_b[:, c, DIN + 32:DIN + 64], in0=stage[:, 1936:1968], scalar1=gin[:, c:c + 1], scalar2=None,
                                                   op0=ALU.mult), reads=[b_stage, b_w], writes=[b_w])
    for c in range(2):
        P.dma("sp", "d_st", E.dma_start(out=stage[:, 0:768], in_=w_qb[0, c * 128:(c + 1) * 128, :]), writes=[b_stage])
        P.op("dve", E.tensor_scalar(out=w_qb_b[:, c, 0:768], in0=stage[:, 0:768], scalar1=gqn[:, c:c + 1], scalar2=SCALE,
                                                   op0=ALU.mult, op1=ALU.mult), reads=[b_stage, b_w], writes=[b_w])
        for h in range(4):
            base = h * 192 + 128
            P.op("dve", E.tensor_scalar(out=w_qb_b[:, c, 768 + h * 64:768 + h * 64 + 32], in0=stage[:, base + 32:base + 64],
                                                                        scalar1=gqn[:, c:c + 1], scalar2=-SCALE, op0=ALU.mult, op1=ALU.mult),
                 reads=[b_stage, b_w], writes=[b_w])
            P.op("dve", E.tensor_scalar(out=w_qb_b[:, c, 768 + h * 64 + 32:768 + h * 64 + 64], in0=stage[:, base:base + 32],
                                                                        scalar1=gqn[:, c:c + 1], scalar2=SCALE, op0=ALU.mult, op1=ALU.mult),
                 reads=[b_stage, b_w], writes=[b_w])
    P.dma("sp", "d_st", E.dma_start(out=stage[:, 0:1024], in_=w_kvb[0]), writes=[b_stage])
    P.op("dve", E.tensor_scalar(out=w_kvb_b, in0=stage[:, 0:1024], scalar1=gkv[:, 0:1], scalar2=None, op0=ALU.mult),
         reads=[b_stage, b_w], writes=[b_w])
    P.dma("sp", "d_st", E.dma_start(out=stage[0:16, 0:256], in_=w_a2[0]), writes=[b_stage])
    P.dma("sp", "d_st", E.dma_start(out=stage[16:17, 0:256], in_=b_a), writes=[b_stage])
    P.op("dve", E.tensor_copy(out=w_a2_b[0:32, :], in_=stage[0:32, 0:256]), reads=[b_stage], writes=[b_w])
    ar.release()
    P.barrier()
    WA = [b_w]

    KnT = ar.alloc([4, LP], BF16)
    KrT = ar.alloc([LP], BF16)
    Vt = ar.alloc([T, 512], BF16)
    b_K = [Buf("K%d" % g) for g in range(NG + 1)]
    uT = ar.alloc([8, 512], BF16); b_uT = Buf("uT")
    mixT = ar.alloc([8, 512], BF16); b_mix = Buf("mixT")
    qkT = ar.alloc([4, 512], F32); b_qk = Buf("qkT")
    silr = ar.alloc([4, 512], BF16); b_silr = Buf("silr")
    alowT = ar.alloc([512], BF16); b_alow = Buf("alowT")
    qlatT = ar.alloc([2, 512], BF16); b_qlat = Buf("qlatT")
    sqq = ar.alloc([2, 512], BF16); b_sqq = Buf("sqq")
    kvlatT = ar.alloc([512], BF16); b_kvlat = Buf("kvlatT")
    sqkv = ar.alloc([512], BF16); b_sqkv = Buf("sqkv")
    QnT = ar.alloc([4, 512], BF16); b_Qn = Buf("QnT")
    QrT = ar.alloc([4, 512], BF16); b_Qr = Buf("QrT")
    cs = ar.alloc([2, 512], F32); b_cs = Buf("cs")
    rq_bc = ar.alloc([512], F32); b_rq = Buf("rq")
    rkv_bc = ar.alloc([512], F32); b_rkv = Buf("rkv")
    rt1 = ar.alloc([512], F32); b_rt1 = Buf("rt1")
    rt2 = ar.alloc([512], F32); b_rt2 = Buf("rt2")
    xt = ar.alloc([D], F32); b_xt = Buf("xt")
    xsb = ar.alloc([D], BF16); b_xsb = Buf("xsb")
    junk = xsb; b_junk = b_xsb
    b_gla = Buf("mixT_gla")
    ssA = ar.alloc([4], F32); b_ssA = Buf("ssA")
    ktok = ar.alloc([256], F32); b_ktok = Buf("ktok")
    vtok = ar.alloc([512], BF16); b_vtok = Buf("vtok")
    la_e = ar.alloc([256], F32); b_lae = Buf("la_e")
    la = ar.alloc([256], F32); b_la = Buf("la")
    ebT = ar.alloc([2, 128], F32); b_eb = Buf("ebT")
    enbT = ar.alloc([2, 128], F32); b_enb = Buf("enbT")
    esuf = ar.alloc([256], F32); b_esuf = Buf("esuf")
    qeT = ar.alloc([2, 128], BF16); b_qe = Buf("qeT")
    keTm = [ar.alloc([2, 128], BF16) for _ in range(2)]; b_ke = Buf("keT")
    kum = [ar.alloc([256], BF16) for _ in range(2)]; b_ku = Buf("ku")
    attm = ar.alloc([4, 128], BF16); b_attm = Buf("attm")
    Sf = ar.alloc([2, 128], F32); b_S = Buf("S")
    Sbm = [ar.alloc([2, 128], BF16) for _ in range(2)]; b_Sb = Buf("Sb")
    SbBm = [ar.alloc([2, 128], BF16) for _ in range(2)]; b_SbB = Buf("SbB")
    ost = ar.alloc([4, 128], F32); b_ost = Buf("ost")
    oT = ar.alloc([4, 128], F32); b_oT = Buf("oT")
    osq = ar.alloc([4, 128], BF16); b_osq = Buf("osq")
    orr = rt1.rearrange("p (a b) -> p a b", a=4); b_orr = b_rt1
    PT = [ar.alloc([512], BF16) for _ in range(3)]
    b_PT = [Buf("PT%d" % i) for i in range(3)]
    PTd = [ar.alloc([512], BF16) for _ in range(4)]
    b_PTd = [Buf("PTd%d" % i) for i in range(4)]
    rinv = rt2; b_rinv = b_rt2
    rkvt = ar.alloc([1], F32); b_rkvt = Buf("rkvt")
    b_mixs = [Buf("mixs%d" % i) for i in range(NSEQ * (NG + 1))]

    pool(E.memset(alowT[0:32, :], 1.0), writes=[b_alow])
    for i in range(2):
        pool(E.memset(keTm[i], 0.0), writes=[b_ke])
        pool(E.memset(kum[i], 0.0), writes=[b_ku])
        pool(E.memset(SbBm[i], 0.0), writes=[b_SbB])
    pool(E.memset(KrT, 0.0), writes=b_K)
    pool(E.memset(QrT, 0.0), writes=[b_Qr])
    for d in range(4):
        pool(E.memset(PTd[d], 0.0), writes=[b_PTd[d]])

    pt_rr = [0]
    st_rr = [0]

    for s in range(NSEQ):
        pool(E.memset(Sf, 0.0), writes=[b_S])
        for i in range(2):
            pool(E.memset(Sbm[i], 0.0), writes=[b_Sb])
        for gi in range(NG + 1):
            ntl = 1 if gi == 0 else 4
            NTOK = 128 * ntl
            tl0 = 0 if gi == 0 else 1 + 4 * (gi - 1)
            c0 = 128 * tl0
            P.dma("sp", "d_cs", E.dma_start(out=cs[0:64, :, 0:NTOK], in_=rope[:, :, c0:c0 + NTOK]), writes=[b_cs])
            for j in range(ntl):
                tl = tl0 + j
                if tl == 0:
                    pool(E.memset(xt, 0.0), writes=[b_xt])
                    P.dma("sp", "d_x", E.dma_start(out=xt[112:128, :], in_=meta), writes=[b_xt])
                else:
                    r0 = (tl - 1) * 128
                    P.dma("sp", "d_x", E.dma_start(out=xt, in_=x[s, r0:r0 + 128, :]), writes=[b_xt])
                P.op("act", E.activation(out=junk, in_=xt, func=AF.Square, accum_out=ssA[:, 0:1]), reads=[b_xt], writes=[b_junk, b_ssA])
                rstd_from_ss(ssA[:, 0:1], 1.0 / D, b_ssA)
                P.op("dve", E.tensor_scalar(out=xsb, in0=xt, scalar1=ssA[:, 0:1], scalar2=None, op0=ALU.mult),
                     reads=[b_xt, b_ssA], writes=[b_xsb])
                bk, bb = gbank()
                bkb = bk.bitcast(BF16)
                for k in range(8):
                    P.op("pe", E.transpose(out=bkb[:, k * 128:(k + 1) * 128], in_=xsb[:, k * 128:(k + 1) * 128], identity=identb),
                         reads=[b_xsb] + C, writes=[bb])
                P.op("act", E.copy(out=uT[:, :, j * 128:(j + 1) * 128], in_=bkb.rearrange("p (k t) -> p k t", k=8)),
                     reads=[bb], writes=[b_uT])

            def proj_fm(col0, M, evac):
                bk, bb = gbank()
                for k in range(8):
                    mm(bk[0:M, 0:NTOK], w_in_b[:, k, col0:col0 + M], uT[:, k, 0:NTOK], k == 0, k == 7, [b_uT] + WA, [bb])
                evac(bk[0:M, 0:NTOK], bb)
            for c in range(4):
                eng = "act" if c % 2 == 0 else "dve"
                if eng == "act":
                    proj_fm(c * 128, 128, lambda p_, bb, c=c: P.op("act", E.copy(out=qkT[:, c, 0:NTOK], in_=p_), reads=[bb], writes=[b_qk]))
                else:
                    proj_fm(c * 128, 128, lambda p_, bb, c=c: P.op("dve", E.tensor_copy(out=qkT[:, c, 0:NTOK], in_=p_), reads=[bb], writes=[b_qk]))
            for h in range(4):
                proj_fm(1024 + h * 128, 128, lambda p_, bb, h=h: P.op("act", E.activation(out=silr[:, h, 0:NTOK], in_=p_, func=AF.Silu),
                                                                         reads=[bb], writes=[b_silr]))
            proj_fm(1536, 16, lambda p_, bb: P.op("dve", E.tensor_copy(out=alowT[0:16, 0:NTOK], in_=p_), reads=[bb], writes=[b_alow]))
            for c in range(2):

                def ev(p_, bb, c=c):
                    P.op("dve", E.tensor_copy(out=qlatT[:, c, 0:NTOK], in_=p_), reads=[bb], writes=[b_qlat])
                    P.op("act", E.activation(out=sqq[:, c, 0:NTOK], in_=p_, func=AF.Square), reads=[bb], writes=[b_sqq, bb])
                proj_fm(1552 + c * 128, 128, ev)

            def evkv(p_, bb):
                P.op("dve", E.tensor_copy(out=kvlatT[:, 0:NTOK], in_=p_), reads=[bb], writes=[b_kvlat])
                P.op("act", E.activation(out=sqkv[:, 0:NTOK], in_=p_, func=AF.Square), reads=[bb], writes=[b_sqkv, bb])
            proj_fm(1808, 128, evkv)
            proj_fm(1936, 64, lambda p_, bb: P.op("dve", E.tensor_tensor(out=rt1[0:64, 0:NTOK], in0=p_, in1=cs[0:64, 0, 0:NTOK], op=ALU.mult),
                                                  reads=[bb, b_cs], writes=[b_rt1]))
            proj_fm(2000, 64, lambda p_, bb: P.op("dve", E.tensor_tensor(out=rt2[0:64, 0:NTOK], in0=p_, in1=cs[0:64, 1, 0:NTOK], op=ALU.mult),
                                                  reads=[bb, b_cs], writes=[b_rt2]))
            pool(E.tensor_tensor(out=KrT[0:64, c0:c0 + NTOK], in0=rt1[0:64, 0:NTOK], in1=rt2[0:64, 0:NTOK], op=ALU.add),
                 reads=[b_rt1, b_rt2], writes=[b_K[gi]])

            bk, bb = gbank()
            for c in range(2):
                mm(bk[:, 0:NTOK], onesb, sqq[:, c, 0:NTOK], c == 0, c == 1, [b_sqq] + C, [bb])
            P.op("act", E.activation(out=rq_bc[:, 0:NTOK], in_=bk[:, 0:NTOK], func=AF.Sqrt, bias=epsb, scale=1.0 / 256),
                 reads=[bb] + C, writes=[b_rq])
            P.op("dve", E.reciprocal(out=rq_bc[:, 0:NTOK], in_=rq_bc[:, 0:NTOK]), reads=[b_rq], writes=[b_rq])
            bk, bb = gbank()
            mm(bk[:, 0:NTOK], onesb, sqkv[:, 0:NTOK], True, True, [b_sqkv] + C, [bb])
            P.op("act", E.activation(out=rkv_bc[:, 0:NTOK], in_=bk[:, 0:NTOK], func=AF.Sqrt, bias=epsb, scale=1.0 / 128),
                 reads=[bb] + C, writes=[b_rkv])
            P.op("dve", E.reciprocal(out=rkv_bc[:, 0:NTOK], in_=rkv_bc[:, 0:NTOK]), reads=[b_rkv], writes=[b_rkv])
            for h in range(4):
                bk, bb = gbank()
                for c in range(2):
                    mm(bk[:, 0:NTOK], w_qb_b[:, c, h * 192:h * 192 + 128], qlatT[:, c, 0:NTOK], c == 0, c == 1, [b_qlat] + WA, [bb])
                P.op("dve", E.tensor_tensor(out=QnT[:, h, 0:NTOK], in0=bk[:, 0:NTOK], in1=rq_bc[:, 0:NTOK], op=ALU.mult),
                     reads=[bb, b_rq], writes=[b_Qn])
                bk, bb = gbank()
                for c in range(2):
                    mm(bk[0:64, 0:NTOK], w_qb_b[:, c, h * 192 + 128:h * 192 + 192], qlatT[:, c, 0:NTOK], c == 0, c == 1, [b_qlat] + WA, [bb])
                P.op("dve", E.tensor_tensor(out=rt1[0:64, 0:NTOK], in0=bk[0:64, 0:NTOK], in1=cs[0:64, 0, 0:NTOK], op=ALU.mult),
                     reads=[bb, b_cs], writes=[b_rt1])
                bk, bb = gbank()
                for c in range(2):
                    mm(bk[0:64, 0:NTOK], w_qb_b[:, c, 768 + h * 64:768 + h * 64 + 64], qlatT[:, c, 0:NTOK], c == 0, c == 1, [b_qlat] + WA, [bb])
                P.op("dve", E.tensor_tensor(out=rt2[0:64, 0:NTOK], in0=bk[0:64, 0:NTOK], in1=cs[0:64, 1, 0:NTOK], op=ALU.mult),
                     reads=[bb, b_cs], writes=[b_rt2])
                pool(E.tensor_tensor(out=rt1[0:64, 0:NTOK], in0=rt1[0:64, 0:NTOK], in1=rt2[0:64, 0:NTOK], op=ALU.add),
                     reads=[b_rt1, b_rt2], writes=[b_rt1])
                pool(E.tensor_tensor(out=QrT[0:64, h, 0:NTOK], in0=rt1[0:64, 0:NTOK], in1=rq_bc[0:64, 0:NTOK], op=ALU.mult),
                     reads=[b_rt1, b_rq], writes=[b_Qr])
                bk, bb = gbank()
                mm(bk[:, 0:NTOK], w_kvb_b[:, h * 256:h * 256 + 128], kvlatT[:, 0:NTOK], True, True, [b_kvlat] + WA, [bb])
                P.op("dve", E.tensor_tensor(out=KnT[:, h, c0:c0 + NTOK], in0=bk[:, 0:NTOK], in1=rkv_bc[:, 0:NTOK], op=ALU.mult),
                     reads=[bb, b_rkv], writes=[b_K[gi]])

            def gla_gen(j):
                tl = tl0 + j
                tc = slice(j * 128, (j + 1) * 128)
                bk, bb = gbank()
                for k in range(8):
                    mm(bk[:, 0:256], uT[:, k, tc], w_in_b[:, k, 256:512], k == 0, k == 7, [b_uT] + WA, [bb])
                P.op("act", E.copy(out=ktok, in_=bk[:, 0:256]), reads=[bb], writes=[b_ktok])
                bk, bb = gbank()
                for k in range(8):
                    mm(bk, uT[:, k, tc], w_in_b[:, k, 512:1024], k == 0, k == 7, [b_uT] + WA, [bb])
                P.op("dve", E.tensor_copy(out=vtok, in_=bk), reads=[bb], writes=[b_vtok])
                bk, bb = gbank()
                mm(bk[:, 0:256], alowT[0:17, tc], w_a2_b[0:17, :], True, True, [b_alow] + WA, [bb])
                P.op("act", E.activation(out=la_e, in_=bk[:, 0:256], func=AF.Exp, scale=-1.0), reads=[bb], writes=[b_lae])
                P.op("act", E.activation(out=la, in_=la_e, func=AF.Ln, bias=1.0), reads=[b_lae], writes=[b_la])
                yield
                bkT, bbT = gbank()
                for pr in range(2):
                    mm(bkT[:, pr * 128:(pr + 1) * 128], la[:, pr * 128:(pr + 1) * 128], triN, True, True, [b_la] + C, [bbT])
                bkS, bbS = gbank()
                mm(bkS[:, 0:256], triUN, la, True, True, [b_la] + C, [bbS])
                P.op("act", E.activation(out=ebT, in_=bkT[:, 0:256].rearrange("p (a b) -> p a b", a=2), func=AF.Exp),
                     reads=[bbT], writes=[b_eb])
                P.op("act", E.activation(out=enbT, in_=bkT[:, 0:256].rearrange("p (a b) -> p a b", a=2), func=AF.Exp, scale=-1.0),
                     reads=[bbT], writes=[b_enb])
                P.op("act", E.activation(out=esuf, in_=bkS[:, 0:256], func=AF.Exp), reads=[bbS], writes=[b_esuf])
                P.op("dve", E.scalar_tensor_tensor(out=qeT, in0=qkT[:, 0:2, tc], scalar=0.125, in1=ebT, op0=ALU.mult, op1=ALU.mult),
                     reads=[b_qk, b_eb], writes=[b_qe])
                for i in range(2):
                    rr = slice(i * 64, i * 64 + 64)
                    P.op("dve", E.tensor_tensor(out=keTm[i][rr], in0=qkT[rr, 2:4, tc], in1=enbT[rr], op=ALU.mult), reads=[b_qk, b_enb], writes=[b_ke])
                    P.op("dve", E.tensor_tensor(out=kum[i][rr], in0=ktok[rr], in1=esuf[rr], op=ALU.mult), reads=[b_ktok, b_esuf], writes=[b_ku])
                yield
                bkA, bbA = gbank()
                for h in range(4):
                    pr, lo = h // 2, (h % 2) * 64
                    mm(bkA[:, h * 128:(h + 1) * 128], keTm[h % 2][:, pr, :], qeT[:, pr, :], True, True, [b_ke, b_qe], [bbA])
                P.op("dve", E.tensor_tensor(out=attm, in0=bkA.rearrange("p (h c) -> p h c", h=4),
                                                             in1=maskBD.unsqueeze(1).to_broadcast([128, 4, 128]), op=ALU.mult),
                     reads=[bbA] + C, writes=[b_attm])
                yield
                bkO, bbO = gbank()
                for h in range(4):
                    mm(bkO[:, h * 128:(h + 1) * 128], vtok[:, h * 128:(h + 1) * 128], attm[:, h, :], True, True, [b_vtok, b_attm], [bbO])
                bkZ, bbZ = gbank()
                for h in range(4):
                    pr, lo = h // 2, (h % 2) * 64
                    mm(bkZ[:, h * 128:h * 128 + 64], Sbm[h % 2][:, pr, :], qeT[:, pr, 0:64], True, True, [b_Sb, b_qe], [bbZ])
                for ch in range(2):
                    rows = slice(ch * 64, ch * 64 + 64)
                    bkU, bbU = gbank()
                    for h in range(4):
                        pr = h // 2
                        mm(bkU[:, h * 128:(h + 1) * 128], kum[ch][:, pr * 128:(pr + 1) * 128], vtok[:, h * 128:(h + 1) * 128], True, True,
                           [b_ku, b_vtok], [bbU])
                    col = ch * 64 + 63
                    for h in range(4):
                        pr, lo = h // 2, (h % 2) * 64
                        P.op("dve", E.scalar_tensor_tensor(
                            out=Sf[lo:lo + 64, pr, :], in0=Sf[lo:lo + 64, pr, :], scalar=ebT[lo:lo + 64, pr, col:col + 1],
                            in1=bkU[lo:lo + 64, h * 128:(h + 1) * 128], op0=ALU.mult, op1=ALU.add), reads=[bbU, b_eb, b_S], writes=[b_S])
                    if ch == 0:
                        for i in range(2):
                            pool(E.tensor_copy(out=SbBm[i][i * 64:i * 64 + 64], in_=Sf[i * 64:i * 64 + 64]), reads=[b_S], writes=[b_SbB])
                        for h in range(4):
                            pr, lo = h // 2, (h % 2) * 64
                            mm(bkZ[:, h * 128 + 64:h * 128 + 128], SbBm[h % 2][:, pr, :], qeT[:, pr, 64:128], True, True, [b_SbB, b_qe], [bbZ])
                    else:
                        for i in range(2):
                            pool(E.tensor_copy(out=Sbm[i][i * 64:i * 64 + 64], in_=Sf[i * 64:i * 64 + 64]), reads=[b_S], writes=[b_Sb])
                    yield
                P.op("act", E.copy(out=ost, in_=bkZ.rearrange("p (h c) -> p h c", h=4)), reads=[bbZ], writes=[b_ost])
                P.op("dve", E.tensor_tensor(out=oT, in0=bkO.rearrange("p (h c) -> p h c", h=4), in1=ost, op=ALU.add),
                     reads=[bbO, b_ost], writes=[b_oT])
                P.op("act", E.activation(out=osq, in_=oT, func=AF.Square), reads=[b_oT], writes=[b_osq])
                yield
                bk, bb = gbank()
                mm(bk, onesb, osq.rearrange("p h c -> p (h c)"), True, True, [b_osq] + C, [bb])
                P.op("act", E.activation(out=orr, in_=bk.rearrange("p (h c) -> p h c", h=4), func=AF.Sqrt, bias=epsb, scale=1.0 / 128),
                     reads=[bb] + C, writes=[b_orr])
                P.op("dve", E.reciprocal(out=orr, in_=orr), reads=[b_orr], writes=[b_orr])
                P.op("dve", E.tensor_tensor(out=oT, in0=oT, in1=orr, op=ALU.mult), reads=[b_oT, b_orr], writes=[b_oT])
                if j == ntl - 1:
                    pass
                P.op("dve", E.tensor_tensor(out=osq, in0=oT, in1=silr[:, :, tc], op=ALU.mult), reads=[b_oT, b_silr], writes=[b_osq])
                P.op("act", E.copy(out=mixT[:, 0:4, j * 128:(j + 1) * 128], in_=osq), reads=[b_osq], writes=[b_gla])
                yield

            for j in range(ntl):
                tl = tl0 + j
                tc = slice(j * 128, (j + 1) * 128)
                bk, bb = gbank()
                mm(bk[:, 0:8], sqkv[:, tc], onesb[:, 0:8], True, True, [b_sqkv] + C, [bb])
                P.op("act", E.activation(out=rkvt, in_=bk[:, 0:1], func=AF.Sqrt, bias=epsb, scale=1.0 / 128),
                     reads=[bb] + C, writes=[b_rkvt])
                P.op("dve", E.reciprocal(out=rkvt, in_=rkvt), reads=[b_rkvt], writes=[b_rkvt])
                bk, bb = gbank()
                for h in range(4):
                    mm(bk[:, h * 128:(h + 1) * 128], kvlatT[:, tc], w_kvb_b[:, h * 256 + 128:h * 256 + 256], True, True, [b_kvlat] + WA, [bb])
                P.op("act", E.activation(out=Vt[:, tl, :], in_=bk, func=AF.Copy, scale=rkvt[:, 0:1]),
                     reads=[bb, b_rkvt], writes=[b_K[gi]])
            gla_iter = itertools.chain(*[gla_gen(j) for j in range(ntl)])


            nfull = 1 if gi == 0 else 4 * gi - 3
            for h in range(4 if gi > 0 else 0):
                bO, bR = ps[:, 2, :], ps[:, 3, :]
                kblocks = [(kt, None) for kt in range(nfull)]
                if gi > 0:
                    kblocks += [(tl0 + d, d) for d in range(4)]
                nkb = len(kblocks)
                def emit_qk(bi):
                    kt, d = kblocks[bi]
                    kg = 0 if kt == 0 else 1 + (kt - 1) // 4
                    q0 = 0 if d is None else 128 * d
                    si = st_rr[0] % 2
                    st_rr[0] += 1
                    bS, bbS_ = ps[:, si, :], psb[si]
                    mm(bS[:, q0:NTOK], KnT[:, h, kt * 128:(kt + 1) * 128], QnT[:, h, q0:NTOK], True, False, [b_K[kg], b_Qn], [bbS_])
                    mm(bS[:, q0:NTOK], KrT[:, kt * 128:(kt + 1) * 128], QrT[:, h, q0:NTOK], False, True, [b_K[kg], b_Qr], [bbS_])
                    return kt, d, kg, q0, bS, bbS_

                def emit_rest(bi, info):
                    kt, d, kg, q0, bS, bbS_ = info
                    if d is None:
                        pi = pt_rr[0] % 3
                        pt_rr[0] += 1
                        pt, bpt = PT[pi], b_PT[pi]
                        if kt == 0:
                            P.op("act", E.activation(out=pt[:, 0:NTOK], in_=bS[:, 0:NTOK], func=AF.Exp, bias=padbias),
                                 reads=[bbS_] + C, writes=[bpt])
                        else:
                            P.op("act", E.activation(out=pt[:, 0:NTOK], in_=bS[:, 0:NTOK], func=AF.Exp), reads=[bbS_], writes=[bpt])
                    else:
                        pt, bpt = PTd[d], b_PTd[d]
                        P.op("act", E.activation(out=pt[0:64, q0:512], in_=bS[0:64, q0:512], func=AF.Exp),
                             reads=[bbS_], writes=[bpt])
                        P.op("act", E.activation(out=pt[64:128, q0 + 64:512], in_=bS[64:128, q0 + 64:512], func=AF.Exp),
                             reads=[bbS_], writes=[bpt])
                    mm(bO[:, q0:NTOK], Vt[:, kt, h * 128:(h + 1) * 128], pt[:, q0:NTOK], bi == 0, bi == nkb - 1, [b_K[kg], bpt], [psb[2]])
                    mm(bR[:, q0:NTOK], onesb, pt[:, q0:NTOK], bi == 0, bi == nkb - 1, [bpt] + C, [psb[3]])

                info = emit_qk(0)
                for bi in range(nkb):
                    nxt = emit_qk(bi + 1) if bi + 1 < nkb else None
                    emit_rest(bi, info)
                    info = nxt
                    if bi % 2 == 1:
                        next(gla_iter, None)
                P.op("dve", E.reciprocal(out=rinv[:, 0:NTOK], in_=bR[:, 0:NTOK]), reads=[psb[3]], writes=[b_rinv])
                P.op("dve", E.tensor_tensor(out=mixT[:, 4 + h, 0:NTOK], in0=bO[:, 0:NTOK], in1=rinv[:, 0:NTOK], op=ALU.mult),
                     reads=[psb[2], b_rinv], writes=[b_mix])
            for _ in gla_iter:
                pass
            gidx = s * (NG + 1) + gi
            if gi > 0:
                P.dma("sp", "d_mix", E.dma_start(out=mixs[gidx], in_=mixT.rearrange("p a b -> p (a b)")), reads=[b_mix, b_gla], writes=[b_mixs[gidx]])

    P.barrier()
    ar.release()

    if phases < 2:
        return _finish(nc, st, P)
    ar.mark()
    w_out_b = ar.alloc([8, D], BF16)
    wr = ar.alloc([8, 72], F32)
    rb_bc = ar.alloc([72], F32)
    g2_bc = ar.alloc([D], F32)
    gon = ar.alloc([4], F32)
    tot = ar.alloc([64], F32); b_tot = Buf("tot")
    lim = ar.alloc([64], F32)
    toti = ar.alloc([64], I32)
    b_w2 = Buf("weightsA2")
    stage2 = ar.alloc([D], F32)
    with nc.allow_non_contiguous_dma(reason="tiny norm-gain vectors / router weights"):
        P.dma("sp", "d_w", E.dma_start(out=gon, in_=gla_on.rearrange("o (c p) -> p (o c)", p=128)), writes=[b_w2])
        P.dma("sp", "d_w", E.dma_start(out=wr[:, :, 0:8], in_=rgw[0].rearrange("(c p) n -> p c n", p=128)), writes=[b_w2])
        P.dma("sp", "d_w", E.dma_start(out=wr[:, :, 8:72], in_=rew[0].rearrange("(c p) n -> p c n", p=128)), writes=[b_w2])
        P.dma("sp", "d_w", E.dma_start(out=rb_bc[:, 0:8], in_=rgb.to_broadcast([128, 8])), writes=[b_w2])
        P.dma("sp", "d_w", E.dma_start(out=rb_bc[:, 8:72], in_=reb.to_broadcast([128, 64])), writes=[b_w2])
        P.dma("sp", "d_w", E.dma_start(out=g2_bc, in_=ffn_norm.to_broadcast([128, D])), writes=[b_w2])
    for c in range(8):
        P.dma("sp", "d_st", E.dma_start(out=stage2, in_=w_out[0, c * 128:(c + 1) * 128, :]), writes=[b_stage])
        if c < 4:
            P.op("dve", E.tensor_scalar(out=w_out_b[:, c, :], in0=stage2, scalar1=gon[:, c:c + 1], scalar2=None, op0=ALU.mult),
                 reads=[b_stage, b_w2], writes=[b_w2])
        else:
            P.op("dve", E.tensor_copy(out=w_out_b[:, c, :], in_=stage2), reads=[b_stage], writes=[b_w2])
    pool(E.iota(toti, pattern=[[CAP, 64]], base=0, channel_multiplier=0), writes=[b_tot])
    pool(E.tensor_copy(out=tot, in_=toti), reads=[b_tot], writes=[b_tot])
    pool(E.tensor_scalar(out=lim, in0=tot, scalar1=float(CAP - 1), scalar2=None, op0=ALU.add), reads=[b_tot], writes=[b_w2])
    W2 = [b_w2]

    mixg = ar.alloc([8, 512], BF16); b_mixg = Buf("mixg")
    u2b = [ar.alloc([D], BF16) for _ in range(2)]; b_u2b = [Buf("u2b0"), Buf("u2b1")]
    _sets = []
    for _i in range(2):
        _sets.append(dict(
            xt2=ar.alloc([D], F32), b_xt2=Buf("xt2_%d" % _i), hT=ar.alloc([D], F32), b_h=Buf("h_%d" % _i),
            junk2=ar.alloc([D], BF16), b_junk2=Buf("junk2_%d" % _i), u2f=ar.alloc([D], F32), b_u2f=Buf("u2f_%d" % _i),
            u2T=ar.alloc([8, 128], F32), b_u2T=Buf("u2T_%d" % _i), ss2=ar.alloc([1], F32), b_ss2=Buf("ss2_%d" % _i),
            lg=ar.alloc([72], F32), b_lg=Buf("lg_%d" % _i), sm=ar.alloc([64], F32), b_sm=Buf("sm_%d" % _i),
            t64=ar.alloc([64], F32), b_t64=Buf("t64_%d" % _i), A1=ar.alloc([64], F32), b_A1=Buf("A1_%d" % _i),
            A2=ar.alloc([64], F32), b_A2=Buf("A2_%d" % _i), Ab=ar.alloc([64], BF16), b_Ab=Buf("Ab_%d" % _i),
            posc=ar.alloc([64], F32), b_posc=Buf("posc_%d" % _i)))
    b_XS = Buf("XS")
    b_out = [Buf("out%d" % i) for i in range(NT)]
    zt = ar.alloc([8192], BF16); b_zt = Buf("zt")
    pool(E.memset(zt, 0.0), writes=[b_zt])
    RPP = NSLOT // 128
    XSz = XS.rearrange("(p r) d -> p (r d)", p=128)
    for c in range(RPP // 8):
        P.dma("sp", "d_z", E.dma_start(out=XSz[:, c * 8192:(c + 1) * 8192], in_=zt), reads=[b_zt], writes=[b_XS])

    for s in range(NSEQ):
        for g in range(1, NG + 1):
            gidx = s * (NG + 1) + g
            P.dma("sp", "d_mg", E.dma_start(out=mixg.rearrange("p a b -> p (a b)"), in_=mixs[gidx]), reads=[b_mixs[gidx]], writes=[b_mixg])
            for j in range(4):
                ti = s * 4 * NG + (g - 1) * 4 + j
                _d = _sets[ti % 2]
                xt2, b_xt2, hT, b_h, junk2, b_junk2, u2f, b_u2f = _d["xt2"], _d["b_xt2"], _d["hT"], _d["b_h"], _d["junk2"], _d["b_junk2"], _d["u2f"], _d["b_u2f"]
                u2T, b_u2T, ss2, b_ss2, lg, b_lg, sm, b_sm = _d["u2T"], _d["b_u2T"], _d["ss2"], _d["b_ss2"], _d["lg"], _d["b_lg"], _d["sm"], _d["b_sm"]
                t64, b_t64, A1, b_A1, A2, b_A2, Ab, b_Ab, posc, b_posc = _d["t64"], _d["b_t64"], _d["A1"], _d["b_A1"], _d["A2"], _d["b_A2"], _d["Ab"], _d["b_Ab"], _d["posc"], _d["b_posc"]
                r0 = (g - 1) * 512 + j * 128
                tc = slice(j * 128, (j + 1) * 128)
                P.dma("sp", "d_x2_%d" % (ti % 2), E.dma_start(out=xt2, in_=x[s, r0:r0 + 128, :]), writes=[b_xt2])
                for half in range(2):
                    bk, bb = gbank()
                    for k in range(8):
                        mm(bk, mixg[:, k, tc], w_out_b[:, k, half * 512:(half + 1) * 512], k == 0, k == 7, [b_mixg] + W2, [bb])
                    P.op("dve", E.tensor_tensor(out=hT[:, half * 512:(half + 1) * 512], in0=bk,
                                                                           in1=xt2[:, half * 512:(half + 1) * 512], op=ALU.add),
                         reads=[bb, b_xt2], writes=[b_h])
                P.dma("sp", "d_h%d" % (ti % 2), E.dma_start(out=out[ti * 128:(ti + 1) * 128, :], in_=hT), reads=[b_h], writes=[b_out[ti]])
                P.op("act", E.activation(out=junk2, in_=hT, func=AF.Square, accum_out=ss2), reads=[b_h], writes=[b_junk2, b_ss2])
                rstd_from_ss(ss2, 1.0 / D, b_ss2)
                P.op("dve", E.scalar_tensor_tensor(out=u2f, in0=hT, scalar=ss2[:, 0:1], in1=g2_bc, op0=ALU.mult, op1=ALU.mult),
                     reads=[b_h, b_ss2] + W2, writes=[b_u2f])
                ub, bub = u2b[ti % 2], b_u2b[ti % 2]
                P.op("act", E.copy(out=ub, in_=u2f), reads=[b_u2f], writes=[bub])
                for half in range(2):
                    bk, bb = gbank()
                    for k in range(4):
                        kk = half * 4 + k
                        P.op("pe", E.transpose(out=bk[:, k * 128:(k + 1) * 128], in_=u2f[:, kk * 128:(kk + 1) * 128], identity=identf),
                             reads=[b_u2f] + C, writes=[bb])
                    P.op("act", E.copy(out=u2T[:, half * 4:half * 4 + 4, :], in_=bk.rearrange("p (k t) -> p k t", k=4)),
                         reads=[bb], writes=[b_u2T])
                bk, bb = gbank()
                for k in range(8):
                    mm(bk[:, 0:72], u2T[:, k, :], wr[:, k, :], k == 0, k == 7, [b_u2T] + W2, [bb])
                P.op("dve", E.tensor_tensor(out=lg, in0=bk[:, 0:72], in1=rb_bc, op=ALU.add), reads=[bb] + W2, writes=[b_lg])
                gl = lg[:, 0:8]
                el3 = lg[:, 8:72].rearrange("p (g e) -> p g e", g=8)
                m8g, m8e = sm[:, 0:8], sm[:, 8:16]
                ohg, sel, oh1, oh2 = sm[:, 16:24], sm[:, 24:32], sm[:, 32:40], sm[:, 40:48]
                ngm, gsum, dd, tt = sm[:, 48:49], sm[:, 49:50], sm[:, 50:51], sm[:, 51:52]
                s1f, s2f = sm[:, 52:53], sm[:, 53:54]
                eg = sm[:, 56:64]
                R_, W_ = [b_lg, b_sm], [b_sm]

                def dv(fn, reads=R_, writes=W_):
                    P.op("dve", fn, reads=reads, writes=writes)
                dv(E.max(out=m8g, in_=gl))
                dv(E.tensor_scalar(out=ohg, in0=gl, scalar1=m8g[:, 0:1], scalar2=None, op0=ALU.is_equal))
                dv(E.tensor_scalar(out=ngm, in0=m8g[:, 0:1], scalar1=-1.0, scalar2=None, op0=ALU.mult))
                P.op("act", E.activation(out=eg, in_=gl, func=AF.Exp, bias=ngm, accum_out=gsum), reads=R_, writes=W_)
                t3 = t64.rearrange("p (g e) -> p g e", g=8)
                dv(E.tensor_tensor(out=t3, in0=el3, in1=ohg.unsqueeze(2).to_broadcast([128, 8, 8]), op=ALU.mult), writes=[b_t64])
                dv(E.tensor_reduce(out=sel, in_=t64.rearrange("p (g e) -> p e g", g=8), axis=AX.X, op=ALU.add), reads=[b_t64, b_sm])
                dv(E.max(out=m8e, in_=sel))
                dv(E.tensor_scalar(out=oh1, in0=sel, scalar1=m8e[:, 0:1], scalar2=None, op0=ALU.is_equal))
                dv(E.tensor_scalar(out=oh2, in0=sel, scalar1=m8e[:, 1:2], scalar2=None, op0=ALU.is_equal))
                dv(E.tensor_tensor(out=dd, in0=m8e[:, 1:2], in1=m8e[:, 0:1], op=ALU.subtract))
                P.op("act", E.activation(out=dd, in_=dd, func=AF.Exp), reads=R_, writes=W_)
                dv(E.tensor_scalar(out=tt, in0=dd, scalar1=1.0, scalar2=gsum, op0=ALU.add, op1=ALU.mult))
                dv(E.reciprocal(out=gates[:, ti, 0:1], in_=tt), writes=[b_sm, b_gates[ti]])
                dv(E.tensor_tensor(out=gates[:, ti, 1:2], in0=gates[:, ti, 0:1], in1=dd, op=ALU.mult), writes=[b_sm, b_gates[ti]])
                A13 = A1.rearrange("p (g e) -> p g e", g=8)
                A23 = A2.rearrange("p (g e) -> p g e", g=8)
                dv(E.tensor_tensor(out=A13, in0=ohg.unsqueeze(2).to_broadcast([128, 8, 8]), in1=oh1.unsqueeze(1).to_broadcast([128, 8, 8]), op=ALU.mult),
                   writes=[b_A1])
                dv(E.tensor_tensor(out=A23, in0=ohg.unsqueeze(2).to_broadcast([128, 8, 8]), in1=oh2.unsqueeze(1).to_broadcast([128, 8, 8]), op=ALU.mult),
                   writes=[b_A2])
                dv(E.tensor_tensor(out=Ab, in0=A1, in1=A2, op=ALU.add), reads=[b_A1, b_A2], writes=[b_Ab])
                bkc, bbc = gbank()
                mm(bkc[:, 0:64], Ub, Ab, True, True, [b_Ab] + C, [bbc])
                mm(bkc[:, 64:128], onesb, Ab, True, True, [b_Ab] + C, [bbc])
                dv(E.tensor_tensor(out=posc, in0=bkc[:, 0:64], in1=tot, op=ALU.add), reads=[bbc, b_tot], writes=[b_posc])
                dv(E.tensor_tensor(out=posc, in0=posc, in1=lim, op=ALU.min), reads=[b_posc] + W2, writes=[b_posc])
                dv(E.tensor_tensor(out=tot, in0=tot, in1=bkc[:, 64:128], op=ALU.add), reads=[bbc, b_tot, b_posc], writes=[b_tot])
                dv(E.tensor_tensor(out=t64, in0=A1, in1=posc, op=ALU.mult), reads=[b_A1, b_posc], writes=[b_t64])
                dv(E.tensor_reduce(out=s1f, in_=t64, axis=AX.X, op=ALU.add), reads=[b_t64, b_sm])
                dv(E.tensor_tensor(out=t64, in0=A2, in1=posc, op=ALU.mult), reads=[b_A2, b_posc, b_sm], writes=[b_t64])
                dv(E.tensor_reduce(out=s2f, in_=t64, axis=AX.X, op=ALU.add), reads=[b_t64, b_sm])
                dv(E.tensor_copy(out=slots[:, ti, 0:2], in_=sm[:, 52:54]), writes=[b_sm, b_gates[ti]])
                for kk in range(2):
                    P.dma("pool", "d_sc%d" % (ti % 2), E.indirect_dma_start(
                        out=XS, out_offset=bass.IndirectOffsetOnAxis(ap=slots[:, ti, kk:kk + 1], axis=0), in_=ub, in_offset=None),
                        reads=[bub, b_gates[ti]], writes=[b_XS])
    P.barrier()
    ar.release()

    if dbg:
        dg = nc.dram_tensor('dbg_gates', [128, NT * 2], F32, kind='ExternalOutput').ap()
        dsl = nc.dram_tensor('dbg_slots', [128, NT * 2], I32, kind='ExternalOutput').ap()
        P.dma('sp', 'd_dbg', E.dma_start(out=dg, in_=gates.rearrange('p a b -> p (a b)')), reads=b_gates)
        P.dma('sp', 'd_dbg', E.dma_start(out=dsl, in_=slots.rearrange('p a b -> p (a b)')), reads=b_gates)
        P.barrier()
    if phases < 3:
        return _finish(nc, st, P)
    ar.mark()
    NBUF = 2
    wst = [[ar.alloc([8, 512], F32), ar.alloc([8, 512], F32), ar.alloc([4, D], F32)] for _ in range(NBUF)]
    b_wst = [[Buf("wst%d_%d" % (i, j)) for j in range(3)] for i in range(NBUF)]
    wbf = [[ar.alloc([8, 512], BF16), ar.alloc([8, 512], BF16), ar.alloc([4, D], BF16)] for _ in range(NBUF)]
    b_wbf = [[Buf("wbf%d_%d" % (i, j)) for j in range(3)] for i in range(NBUF)]
    xsl = [ar.alloc([D], BF16) for _ in range(2)]; b_xsl = [Buf("xsl0"), Buf("xsl1")]
    XT = [ar.alloc([8, CAP], BF16) for _ in range(2)]; b_XT = [Buf("XT0"), Buf("XT1")]
    sg = [ar.alloc([CAP], F32) for _ in range(2)]; b_sg = [Buf("sg0"), Buf("sg1")]
    hTb = [ar.alloc([4, CAP], BF16) for _ in range(2)]; b_hTb = [Buf("hTb0"), Buf("hTb1")]
    ybuf = [ar.alloc([D], F32) for _ in range(2)]; b_ybuf = [Buf("ybuf0"), Buf("ybuf1")]
    b_YS = Buf("YS")
    yrr = [0]
    xrr = [0]

    def load_expert(ex):
        i = ex % NBUF
        P.dma("sp", "d_wg%d" % i, E.dma_start(out=wst[i][0], in_=ewg[0, ex].rearrange("(c p) n -> p c n", p=128)), writes=[b_wst[i][0]])
        P.dma("sp", "d_wu%d" % i, E.dma_start(out=wst[i][1], in_=ewu[0, ex].rearrange("(c p) n -> p c n", p=128)), writes=[b_wst[i][1]])
        P.dma("sp", "d_wd%d" % i, E.dma_start(out=wst[i][2], in_=ewd[0, ex].rearrange("(c p) n -> p c n", p=128)), writes=[b_wst[i][2]])

    def cast_expert(ex):
        i = ex % NBUF
        P.op("dve", E.tensor_copy(out=wbf[i][0], in_=wst[i][0]), reads=[b_wst[i][0]], writes=[b_wbf[i][0]])
        P.op("pool", E.tensor_copy(out=wbf[i][1], in_=wst[i][1]), reads=[b_wst[i][1]], writes=[b_wbf[i][1]])
        P.op("act", E.copy(out=wbf[i][2], in_=wst[i][2]), reads=[b_wst[i][2]], writes=[b_wbf[i][2]])

    load_expert(0)
    for ex in range(NE):
        i = ex % NBUF
        cast_expert(ex)
        if ex + 1 < NE:
            load_expert(ex + 1)
        xi = ex % 2
        for sb in range(NB):
            xs_i = xrr[0] % 2
            xrr[0] += 1
            r0 = ex * CAP + sb * 128
            P.dma("act", "d_xs%d" % xs_i, E.dma_start(out=xsl[xs_i], in_=XS[r0:r0 + 128, :]), reads=[b_XS], writes=[b_xsl[xs_i]])
            bk, bb = gbank()
            bkb = bk.bitcast(BF16)
            for k in range(8):
                P.op("pe", E.transpose(out=bkb[:, k * 128:(k + 1) * 128], in_=xsl[xs_i][:, k * 128:(k + 1) * 128], identity=identb),
                     reads=[b_xsl[xs_i]] + C, writes=[bb])
            P.op("dve", E.tensor_copy(out=XT[xi][:, :, sb * 128:(sb + 1) * 128], in_=bkb.rearrange("p (k t) -> p k t", k=8)),
                 reads=[bb], writes=[b_XT[xi]])
        for jh in range(4):
            hi = jh % 2
            bkG, bbG = gbank()
            for k in range(8):
                mm(bkG[:, 0:CAP], wbf[i][0][:, k, jh * 128:(jh + 1) * 128], XT[xi][:, k, :], k == 0, k == 7, [b_wbf[i][0], b_XT[xi]], [bbG])
            bkU, bbU = gbank()
            for k in range(8):
                mm(bkU[:, 0:CAP], wbf[i][1][:, k, jh * 128:(jh + 1) * 128], XT[xi][:, k, :], k == 0, k == 7, [b_wbf[i][1], b_XT[xi]], [bbU])
            P.op("act", E.activation(out=sg[hi], in_=bkG[:, 0:CAP], func=AF.Silu), reads=[bbG], writes=[b_sg[hi]])
            P.op("dve", E.tensor_tensor(out=hTb[xi][:, jh, :], in0=bkU[:, 0:CAP], in1=sg[hi], op=ALU.mult),
                 reads=[bbU, b_sg[hi]], writes=[b_hTb[xi]])
        for sb in range(NB):
            yi = yrr[0] % 2
            yrr[0] += 1
            for half in range(2):
                bk, bb = gbank()
                for jh in range(4):
                    mm(bk, hTb[xi][:, jh, sb * 128:(sb + 1) * 128], wbf[i][2][:, jh, half * 512:(half + 1) * 512], jh == 0, jh == 3,
                       [b_hTb[xi], b_wbf[i][2]], [bb])
                if half == 0:
                    P.op("act", E.copy(out=ybuf[yi][:, 0:512], in_=bk), reads=[bb], writes=[b_ybuf[yi]])
                else:
                    P.op("dve", E.tensor_copy(out=ybuf[yi][:, 512:1024], in_=bk), reads=[bb], writes=[b_ybuf[yi]])
            r0 = ex * CAP + sb * 128
            P.dma("sp", "d_ys%d" % yi, E.dma_start(out=YS[r0:r0 + 128, :], in_=ybuf[yi]), reads=[b_ybuf[yi]], writes=[b_YS])
    P.barrier()
    ar.release()

    if phases < 4:
        return _finish(nc, st, P)
    ar.mark()
    gf_bc = ar.alloc([D], F32); b_gf = Buf("gf")
    with nc.allow_non_contiguous_dma(reason="broadcast norm gain"):
        P.dma("sp", "d_w", E.dma_start(out=gf_bc, in_=final_norm.to_broadcast([128, D])), writes=[b_gf])
    hb = [ar.alloc([D], F32) for _ in range(2)]; b_hb = [Buf("hb0"), Buf("hb1")]
    y1 = [ar.alloc([D], F32) for _ in range(2)]; b_y1 = [Buf("y1_0"), Buf("y1_1")]
    y2 = [ar.alloc([D], F32) for _ in range(2)]; b_y2 = [Buf("y2_0"), Buf("y2_1")]
    ob = [ar.alloc([D], F32) for _ in range(2)]; b_ob = [Buf("ob0"), Buf("ob1")]
    junk3 = ar.alloc([D], F32); b_junk3 = Buf("junk3")
    ss3 = [ar.alloc([1], F32) for _ in range(2)]; b_ss3 = [Buf("ss3_0"), Buf("ss3_1")]
    for ti in range(NT):
        i = ti % 2
        P.dma("sp", "d_hb%d" % i, E.dma_start(out=hb[i], in_=out[ti * 128:(ti + 1) * 128, :]), reads=[b_out[ti]], writes=[b_hb[i]])
        P.dma("pool", "d_g1%d" % i, E.indirect_dma_start(
            out=y1[i], out_offset=None, in_=YS, in_offset=bass.IndirectOffsetOnAxis(ap=slots[:, ti, 0:1], axis=0)), reads=[b_YS, b_gates[ti]], writes=[b_y1[i]])
        P.dma("pool", "d_g2%d" % i, E.indirect_dma_start(
            out=y2[i], out_offset=None, in_=YS, in_offset=bass.IndirectOffsetOnAxis(ap=slots[:, ti, 1:2], axis=0)), reads=[b_YS, b_gates[ti]], writes=[b_y2[i]])
        P.op("dve", E.scalar_tensor_tensor(out=hb[i], in0=y1[i], scalar=gates[:, ti, 0:1], in1=hb[i], op0=ALU.mult, op1=ALU.add),
             reads=[b_y1[i], b_hb[i], b_gates[ti]], writes=[b_hb[i]])
        P.op("dve", E.scalar_tensor_tensor(out=hb[i], in0=y2[i], scalar=gates[:, ti, 1:2], in1=hb[i], op0=ALU.mult, op1=ALU.add),
             reads=[b_y2[i], b_hb[i], b_gates[ti]], writes=[b_hb[i]])
        P.op("act", E.activation(out=junk3, in_=hb[i], func=AF.Square, accum_out=ss3[i]), reads=[b_hb[i]], writes=[b_junk3, b_ss3[i]])
        rstd_from_ss(ss3[i], 1.0 / D, b_ss3[i])
        P.op("dve", E.scalar_tensor_tensor(out=ob[i], in0=hb[i], scalar=ss3[i][:, 0:1], in1=gf_bc, op0=ALU.mult, op1=ALU.mult),
             reads=[b_hb[i], b_ss3[i], b_gf], writes=[b_ob[i]])
        P.dma("sp", "d_o%d" % i, E.dma_start(out=out[ti * 128:(ti + 1) * 128, :], in_=ob[i]), reads=[b_ob[i], b_hb[i]], writes=[b_out[ti]])
    P.barrier()
    ar.release()
    return _finish(nc, st, P)


def _finish(nc, st, P):
    P.barrier()
    P.emit(st)
    st.close()
    return nc, st, P


def rope_table(NG):
    T = 1 + 4 * NG
    LP = 128 * T
    pos = (np.arange(LP, dtype=np.float32) - np.float32(112.0)).astype(np.float32)
    inv = (np.float32(10000.0) ** (-np.arange(0, 64, 2, dtype=np.float32) / np.float32(64))).astype(np.float32)
    ang = (pos[None, :] * inv[:, None]).astype(np.float32)
    cs = np.zeros((64, 2, LP), np.float32)
    cs[0:32, 0] = np.cos(ang); cs[32:64, 0] = np.cos(ang)
    cs[0:32, 1] = np.sin(ang); cs[32:64, 1] = np.sin(ang)
    return cs


_CACHE = {}


def run(inputs, n_cores, NSEQ, NG, CAP, dbg=False, phases=4):
    key = (NSEQ, NG, CAP, dbg, phases)
    if key not in _CACHE:
        _CACHE[key] = build_program(NSEQ, NG, CAP, dbg, phases)
    nc = _CACHE[key]
    x = np.ascontiguousarray(inputs["x"], dtype=np.float32)
    cs = rope_table(NG)
    in_maps = []
    for c in range(n_cores):
        m = {"x": x[c * NSEQ:(c + 1) * NSEQ], "rope_cs": cs}
        for k, v in inputs.items():
            if k == "x":
                continue
            a = np.ascontiguousarray(v, dtype=np.float32)
            if k == "final_norm":
                a = a.reshape(1, -1)
            m[k] = a
        in_maps.append(m)
    res = run_bass_kernel_spmd(nc, in_maps, core_ids=list(range(n_cores)))
    return res


def kernel(**inputs):
    x = inputs["x"]
    B, S, Dm = x.shape
    res = run(inputs, 8, 2, 8, 384)
    outs = [r["out"].reshape(2, S, Dm) for r in res.results]
    return np.concatenate(outs, axis=0).astype(np.float32)
```
